# Optimizing a Trainium2 kernel written in Bass

```python
import math
import jax, jax.numpy as jnp
from jax import lax
import numpy as np

D_MODEL = 1024
BATCH = 8
SEQ = 4096
DEPTH = 2

HEAD_DIM = 64
FOX_HEADS = 8
FOX_WIDTH = FOX_HEADS * HEAD_DIM
MLA_HEADS = 4
MLA_NOPE = 128
MLA_ROPE = 64
MLA_QK = MLA_NOPE + MLA_ROPE
MLA_V = 128
MLA_Q_RANK = 256
MLA_KV_RANK = 128
MLA_WIDTH = MLA_HEADS * MLA_V
MOBA_HEADS = 8
MOBA_WIDTH = MOBA_HEADS * HEAD_DIM
MOBA_BLOCK = 256
MOBA_TOPK = 3
MOBA_Q_CHUNK = 32
D_MIX = FOX_WIDTH + MLA_WIDTH + MOBA_WIDTH
Q_BLOCK = 128
ROPE_THETA = 10000.0
EPS = 1e-6
IN_SIZES = (FOX_WIDTH, FOX_WIDTH, FOX_WIDTH, FOX_HEADS, FOX_WIDTH,
            MLA_Q_RANK, MLA_KV_RANK, MLA_ROPE, MLA_WIDTH,
            MOBA_WIDTH, MOBA_WIDTH, MOBA_WIDTH, MOBA_WIDTH)
D_IN = sum(IN_SIZES)

kernel_name = "hybrid_fox_mla_moba_parallel_heads"


def rmsnorm(x, g):
    xf = x.astype(jnp.float32)
    y = xf * lax.rsqrt(jnp.mean(xf * xf, axis=-1, keepdims=True) + EPS)
    return (y * g.astype(jnp.float32)).astype(x.dtype)


def rope_tables(S):
    inv = ROPE_THETA ** (-jnp.arange(0, MLA_ROPE, 2, dtype=jnp.float32) / MLA_ROPE)
    ang = jnp.arange(S, dtype=jnp.float32)[:, None] * inv[None, :]
    ang = jnp.concatenate([ang, ang], axis=-1)
    return jnp.cos(ang), jnp.sin(ang)


def apply_rope(x, cos, sin):
    x1, x2 = jnp.split(x, 2, axis=-1)
    rot = jnp.concatenate([-x2, x1], axis=-1)
    return (x * cos + rot * sin).astype(x.dtype)


def causal_block_sweep(qh, kh, vh, decay=None):
    B, H, S, Dk = qh.shape
    scale = Dk ** -0.5
    kpos = jnp.arange(S)

    def one_block(i):
        start = i * Q_BLOCK
        qb = lax.dynamic_slice_in_dim(qh, start, Q_BLOCK, axis=2)
        s = jnp.einsum('bhqd,bhkd->bhqk', qb, kh, preferred_element_type=jnp.float32) * scale
        if decay is not None:
            db = lax.dynamic_slice_in_dim(decay, start, Q_BLOCK, axis=2)
            s = s + (db[..., :, None] - decay[..., None, :])
        qpos = start + jnp.arange(Q_BLOCK)
        s = jnp.where(qpos[:, None] >= kpos[None, :], s, -jnp.inf)
        p = jax.nn.softmax(s, axis=-1)
        return jnp.einsum('bhqk,bhkd->bhqd', p.astype(vh.dtype), vh)

    out = lax.map(one_block, jnp.arange(S // Q_BLOCK))
    Dv = out.shape[-1]
    return out.transpose(1, 0, 3, 2, 4).reshape(B, S, H * Dv)


def fox_attention(q, k, v, f_logit, b_f):
    B, S, _ = q.shape
    shp = (B, S, FOX_HEADS, HEAD_DIM)
    qh = q.reshape(shp).transpose(0, 2, 1, 3)
    kh = k.reshape(shp).transpose(0, 2, 1, 3)
    vh = v.reshape(shp).transpose(0, 2, 1, 3)
    log_f = jax.nn.log_sigmoid((f_logit + b_f).astype(jnp.float32))
    c = jnp.cumsum(log_f, axis=1).transpose(0, 2, 1)
    return causal_block_sweep(qh, kh, vh, decay=c)


def mla_attention(cq, ckv, kr, g_q, w_uq, g_kv, w_ukv, cos, sin):
    B, S, _ = cq.shape
    q = (rmsnorm(cq, g_q) @ w_uq).reshape(B, S, MLA_HEADS, MLA_QK)
    q_nope, q_rope = q[..., :MLA_NOPE], q[..., MLA_NOPE:]
    q = jnp.concatenate([q_nope, apply_rope(q_rope, cos[:, None, :], sin[:, None, :])], axis=-1)
    kv = (rmsnorm(ckv, g_kv) @ w_ukv).reshape(B, S, MLA_HEADS, MLA_NOPE + MLA_V)
    k_nope, v = kv[..., :MLA_NOPE], kv[..., MLA_NOPE:]
    k_rope = apply_rope(kr, cos, sin)
    k = jnp.concatenate([k_nope, jnp.broadcast_to(k_rope[:, :, None, :], (B, S, MLA_HEADS, MLA_ROPE)).astype(k_nope.dtype)], axis=-1)
    return causal_block_sweep(q.transpose(0, 2, 1, 3), k.transpose(0, 2, 1, 3), v.transpose(0, 2, 1, 3))


def moba_attention(q, k, v, slopes):
    B, S, _ = q.shape
    H, D, BLK = MOBA_HEADS, HEAD_DIM, MOBA_BLOCK
    shp = (B, S, H, D)
    qh = q.reshape(shp).transpose(0, 2, 1, 3)
    kh = k.reshape(shp).transpose(0, 2, 1, 3)
    vh = v.reshape(shp).transpose(0, 2, 1, 3)
    nb = -(-S // BLK)
    pad = nb * BLK - S
    kb = jnp.pad(kh, ((0, 0), (0, 0), (0, pad), (0, 0))).reshape(B, H, nb, BLK, D)
    vb = jnp.pad(vh, ((0, 0), (0, 0), (0, pad), (0, 0))).reshape(B, H, nb, BLK, D)
    kmean = jnp.mean(kb.astype(jnp.float32), axis=3)
    topk = min(MOBA_TOPK, nb)
    scale = D ** -0.5
    key_off = jnp.arange(BLK)
    bi = jnp.arange(B)[:, None, None, None]
    hi = jnp.arange(H)[None, :, None, None]
    m = slopes.astype(jnp.float32)

    def one_chunk(i):
        start = i * MOBA_Q_CHUNK
        qc = lax.dynamic_slice_in_dim(qh, start, MOBA_Q_CHUNK, axis=2)
        qpos = start + jnp.arange(MOBA_Q_CHUNK)
        cur = start // BLK
        gate = jnp.einsum('bhqd,bhnd->bhqn', qc.astype(jnp.float32), kmean)
        gate = jnp.where(jnp.arange(nb) < cur, gate, -jnp.inf)
        _, sel = lax.top_k(gate, topk)
        valid = sel < cur
        ks = kb[bi, hi, sel]
        vs = vb[bi, hi, sel]
        s_sel = jnp.einsum('bhqd,bhqnkd->bhqnk', qc, ks, preferred_element_type=jnp.float32) * scale
        kpos_sel = sel[..., None] * BLK + key_off
        dist_sel = (qpos[None, None, :, None, None] - kpos_sel).astype(jnp.float32)
        s_sel = s_sel - m[None, :, None, None, None] * dist_sel
        s_sel = jnp.where(valid[..., None], s_sel, -jnp.inf).reshape(B, H, MOBA_Q_CHUNK, topk * BLK)
        ko = lax.dynamic_index_in_dim(kb, cur, axis=2, keepdims=False)
        vo = lax.dynamic_index_in_dim(vb, cur, axis=2, keepdims=False)
        kpos_own = cur * BLK + key_off
        dist_own = (qpos[:, None] - kpos_own[None, :]).astype(jnp.float32)
        s_own = jnp.einsum('bhqd,bhkd->bhqk', qc, ko, preferred_element_type=jnp.float32) * scale
        s_own = s_own - m[None, :, None, None] * dist_own
        s_own = jnp.where(dist_own >= 0, s_own, -jnp.inf)
        p = jax.nn.softmax(jnp.concatenate([s_sel, s_own], axis=-1), axis=-1)
        p_sel = p[..., :topk * BLK].reshape(B, H, MOBA_Q_CHUNK, topk, BLK).astype(vs.dtype)
        p_own = p[..., topk * BLK:].astype(vo.dtype)
        return (jnp.einsum('bhqnk,bhqnkd->bhqd', p_sel, vs)
                + jnp.einsum('bhqk,bhkd->bhqd', p_own, vo))

    out = lax.map(one_chunk, jnp.arange(S // MOBA_Q_CHUNK))
    return out.transpose(1, 0, 3, 2, 4).reshape(B, S, H * D)


def hybrid_layer(x, ln_g, w_in, b_f, g_q, w_uq, g_kv, w_ukv, out_g, w_out, cos, sin, slopes):
    h = rmsnorm(x, ln_g)
    proj = h @ w_in
    cuts, acc = [], 0
    for sz in IN_SIZES[:-1]:
        acc += sz
        cuts.append(acc)
    (fq, fk, fv, ff, fz, cq, ckv, kr, mz, bq, bk, bv, bz) = jnp.split(proj, cuts, axis=-1)
    y_a = fox_attention(fq, fk, fv, ff, b_f)
    y_b = mla_attention(cq, ckv, kr, g_q, w_uq, g_kv, w_ukv, cos, sin)
    y_c = moba_attention(bq, bk, bv, slopes)
    g_a, g_b, g_c = jnp.split(out_g, [FOX_WIDTH, FOX_WIDTH + MLA_WIDTH])
    y = jnp.concatenate([rmsnorm(y_a, g_a) * jax.nn.silu(fz),
                         rmsnorm(y_b, g_b) * jax.nn.silu(mz),
                         rmsnorm(y_c, g_c) * jax.nn.silu(bz)], axis=-1)
    return x + y @ w_out


def setup_inputs(seed: int = 0) -> dict:
    key = jax.random.key(seed)
    ks = jax.random.split(key, 12)
    f32 = jnp.float32
    nrm = lambda k, shp: jax.random.normal(k, shp, dtype=f32)
    return {
        "x": nrm(ks[0], (BATCH, SEQ, D_MODEL)),
        "ln_g": 1.0 + 0.02 * nrm(ks[1], (DEPTH, D_MODEL)),
        "w_in": nrm(ks[2], (DEPTH, D_MODEL, D_IN)) * D_MODEL ** -0.5,
        "fox_b_f": 2.0 + 0.5 * nrm(ks[3], (DEPTH, FOX_HEADS)),
        "mla_q_g": 1.0 + 0.02 * nrm(ks[4], (DEPTH, MLA_Q_RANK)),
        "mla_w_uq": nrm(ks[5], (DEPTH, MLA_Q_RANK, MLA_HEADS * MLA_QK)) * MLA_Q_RANK ** -0.5,
        "mla_kv_g": 1.0 + 0.02 * nrm(ks[6], (DEPTH, MLA_KV_RANK)),
        "mla_w_ukv": nrm(ks[7], (DEPTH, MLA_KV_RANK, MLA_HEADS * (MLA_NOPE + MLA_V))) * MLA_KV_RANK ** -0.5,
        "out_g": 1.0 + 0.02 * nrm(ks[8], (DEPTH, D_MIX)),
        "w_out": nrm(ks[9], (DEPTH, D_MIX, D_MODEL)) * D_MIX ** -0.5,
        "final_g": 1.0 + 0.02 * nrm(ks[10], (D_MODEL,)),
    }


def reference(x, ln_g, w_in, fox_b_f, mla_q_g, mla_w_uq, mla_kv_g, mla_w_ukv, out_g, w_out, final_g):
    S = x.shape[1]
    cos, sin = rope_tables(S)
    slopes = 2.0 ** (-8.0 * jnp.arange(1, MOBA_HEADS + 1, dtype=jnp.float32) / MOBA_HEADS)
    for l in range(DEPTH):
        x = hybrid_layer(x, ln_g[l], w_in[l], fox_b_f[l], mla_q_g[l], mla_w_uq[l],
                         mla_kv_g[l], mla_w_ukv[l], out_g[l], w_out[l], cos, sin, slopes)
    return rmsnorm(x, final_g)
```

```python
import numpy as np
import ml_dtypes
import concourse.bass as bass
import concourse.mybir as mybir
from concourse.bass_utils import run_bass_kernel_spmd

F32 = mybir.dt.float32
BF16 = mybir.dt.bfloat16
AF = mybir.ActivationFunctionType
ALU = mybir.AluOpType
AX = mybir.AxisListType

S = 4096
D = 1024
NCH = 8
NT = 32
DIN = 5064
DINX = 5128
EPS = 1e-6
NEG = -30000.0
C_FQ, C_FK, C_FV, C_FF, C_FZ = 0, 512, 1024, 1536, 1544
C_CQ, C_CKV, C_KR, C_MZ = 2056, 2312, 2440, 2504
C_BQ, C_BK, C_BV, C_BZ = 3016, 3528, 4040, 4552
C_KRS = 5064

ENGS = ('pe', 'act', 'dve', 'pool', 'sp')
SKIP = set()
NCH_RUN = [8]
SAME_ENG_SYNC = True


class _Rec:
    def __init__(self):
        self.call = None

    def __getattr__(self, name):
        def f(*a, **kw):
            self.call = (name, a, kw)
            return self
        return f


def _capture(fn):
    r = _Rec()
    fn(r)
    name, a, kw = r.call
    return lambda eng: getattr(eng, name)(*a, **kw)


class Prog:
    def __init__(self, nc):
        self.nc = nc
        self.ops = {e: [] for e in ENGS}
        self.last_w = {}
        self.readers = {}
        self.dma_n = {}
        self.base = set()
        self.skip = False

    def _deps(self, r, w):
        d = set(self.base)
        for x in r:
            ev = self.last_w.get(x)
            if ev is not None:
                d.add(ev)
        for x in w:
            ev = self.last_w.get(x)
            if ev is not None:
                d.add(ev)
            for ev in self.readers.get(x, {}).values():
                d.add(ev)
        return d

    def _commit(self, ev, r, w):
        for x in r:
            self.readers.setdefault(x, {})[ev[:2]] = ev
        for x in w:
            self.last_w[x] = ev
            self.readers[x] = {}

    def sec(self, name):
        self.skip = name in SKIP

    def op(self, eng, fn, r=(), w=(), ss=False):
        if self.skip:
            return
        idx = len(self.ops[eng])
        ev = ('e', eng, idx)
        deps = self._deps(r, w)
        self.ops[eng].append(dict(fn=_capture(fn), deps=deps, dma=None, ss=ss))
        self._commit(ev, r, w)

    def dma(self, eng, sem, fn, r=(), w=()):
        if self.skip:
            return
        n = self.dma_n.get(sem, 0) + 1
        self.dma_n[sem] = n
        ev = ('d', sem, n)
        deps = self._deps(r, w)
        if n > 1:
            deps.add(('d', sem, n - 1))
        self.ops[eng].append(dict(fn=_capture(fn), deps=deps, dma=sem))
        self._commit(ev, r, w)

    def barrier(self):
        b = set()
        for e in ENGS:
            for idx in range(len(self.ops[e]) - 1, -1, -1):
                if self.ops[e][idx]['dma'] is None:
                    b.add(('e', e, idx))
                    break
        for sem, n in self.dma_n.items():
            b.add(('d', sem, n))
        self.base = b

    def build(self):
        nc = self.nc
        ops = self.ops
        dma_n = self.dma_n
        plan = {e: [] for e in ENGS}
        sig = {e: set() for e in ENGS}
        for e in ENGS:
            waited = {}
            for o in ops[e]:
                need = {}
                for (kind, key, val) in o['deps']:
                    if kind == 'e' and key == e and (e == 'pe' or not (SAME_ENG_SYNC or o.get('ss'))):
                        continue
                    k2 = (kind, key)
                    if waited.get(k2, -1) >= val:
                        continue
                    need[k2] = max(need.get(k2, -1), val)
                for k2, v in need.items():
                    waited[k2] = v
                    if k2[0] == 'e':
                        sig[k2[1]].add(v)
                plan[e].append(need)
        cnt = {e: {idx: i + 1 for i, idx in enumerate(sorted(sig[e]))} for e in ENGS}
        sems = {e: nc.alloc_semaphore('s_' + e) for e in ENGS}
        dsems = {k: nc.alloc_semaphore('d_' + k) for k in self.dma_n}

        def replay(e, eng):
            lastd = {}
            for idx, o in enumerate(ops[e]):
                for k2, v in plan[e][idx].items():
                    if k2[0] == 'e':
                        eng.wait_ge(sems[k2[1]], cnt[k2[1]][v])
                    else:
                        eng.wait_ge(dsems[k2[1]], 16 * v)
                        lastd[k2[1]] = v
                ins = o['fn'](eng)
                if o['dma'] is not None:
                    ins.then_inc(dsems[o['dma']], 16)
                elif idx in cnt[e]:
                    ins.then_inc(sems[e], 1)
            if e == 'sp':
                for k, n in dma_n.items():
                    if lastd.get(k, 0) < n:
                        eng.wait_ge(dsems[k], 16 * n)

        with nc.Block() as block:
            @block.tensor
            def _(eng):
                replay('pe', eng)

            @block.scalar
            def _(eng):
                replay('act', eng)

            @block.vector
            def _(eng):
                replay('dve', eng)

            @block.gpsimd
            def _(eng):
                replay('pool', eng)

            @block.sync
            def _(eng):
                replay('sp', eng)


class Arena:
    def __init__(self, nc, lo, hi):
        self.nc, self.lo, self.hi, self.cur = nc, lo, hi, lo
        self.n = 0

    def alloc(self, name, shape, dtype):
        nbytes = int(np.prod(shape[1:])) * (4 if dtype == F32 else 2)
        nbytes = (nbytes + 63) // 64 * 64
        off = self.cur
        assert off + nbytes <= self.hi, (name, off, nbytes, self.hi)
        self.cur += nbytes
        self.n += 1
        return self.nc.alloc_sbuf_tensor_at(name, list(shape), dtype, offset=off)


def build_program(n_layers=2, dbg=None, phases=('A', 'B', 'C')):
    nc = bass.Bass("TRN2", target_bir_lowering=False)
    P = Prog(nc)
    dbg = dbg or ()

    def dt_in(name, shape, dtype=F32):
        return nc.dram_tensor(name, list(shape), dtype, kind="ExternalInput")

    def dt_scr(name, shape, dtype):
        kind = "ExternalOutput" if name in dbg else "Internal"
        return nc.dram_tensor(name, list(shape), dtype, kind=kind)

    x_in = dt_in("x", [S, D])
    ln_g = dt_in("ln_g", [2, 128, 8])
    w_in = dt_in("w_in", [2, D, DINX])
    b_f = dt_in("fox_b_f", [2, 8, 1])
    q_g = dt_in("mla_q_g", [2, 128, 2])
    w_uq = dt_in("mla_w_uq", [2, 256, 1024])
    kv_g = dt_in("mla_kv_g", [2, 128, 1])
    w_ukv = dt_in("mla_w_ukv", [2, 128, 1024])
    out_g = dt_in("out_g", [2, 128, 12])
    w_out = dt_in("w_out", [2, 1536, D])
    fin_g = dt_in("final_g", [D])
    c_ident = dt_in("c_ident", [128, 128])
    c_tri = dt_in("c_tri", [128, 128], BF16)
    c_cos = dt_in("c_cos", [128, S])
    c_sin = dt_in("c_sin", [128, S])
    c_ones = dt_in("c_ones", [8, 3, S], BF16)
    c_mq = dt_in("c_mq", [8, 6, S], BF16)
    c_mk = dt_in("c_mk", [8, 22, S], BF16)
    y_out = nc.dram_tensor("y", [S, D], F32, kind="ExternalOutput")

    fq_s = dt_scr("fq_s", [8, 70, S], BF16)
    fk_s = dt_scr("fk_s", [8, 70, S], BF16)
    fv_s = dt_scr("fv_s", [S, 8 * 65], BF16)
    bq_s = dt_scr("bq_s", [8, 86, S], BF16)
    bk_s = dt_scr("bk_s", [8, 86, S], BF16)
    bv_s = dt_scr("bv_s", [S, 8 * 65], BF16)
    mq1_s = dt_scr("mq1_s", [4, 128, S], BF16)
    mq2_s = dt_scr("mq2_s", [4, 64, S], BF16)
    mk1_s = dt_scr("mk1_s", [4, 128, S], BF16)
    mk2_s = dt_scr("mk2_s", [64, S], BF16)
    mv_s = dt_scr("mv_s", [S, 4 * 129], BF16)
    gz_s = dt_scr("gz_s", [S, 1536], F32)
    yy_s = dt_scr("yy_s", [S, 1536], BF16)
    xr_s = dt_scr("xr_s", [S, D], F32)

    LO, HI = 16512 + 64, 229344
    AP_ = Arena(nc, LO, HI)
    ident = AP_.alloc("ident", [128, 128], F32)
    identb = AP_.alloc("identb", [128, 128], BF16)
    tri = AP_.alloc("tri", [128, 128], BF16)
    onesf = AP_.alloc("onesf", [128, 128], F32)
    epsc = AP_.alloc("epsc", [128, 1], F32)
    kmT = AP_.alloc("kmT", [128, 4, 2, 16], F32)
    persist_end = AP_.cur
    WH = DINX // 2
    WA = Arena(nc, persist_end, HI)
    Wb = WA.alloc("Wb", [128, 8, DINX], BF16)
    wst = WA.alloc("wst", [128, WH], F32)
    gcol = WA.alloc("gcol", [128, 8], F32)
    wend = WA.cur
    TOPLO = HI - (12 * D * 2 + D * 4 + 64 + 64)
    TA = Arena(nc, TOPLO, HI)
    Wo = TA.alloc("Wo", [128, 12, D], BF16)
    wstC = TA.alloc("wstC", [128, D], F32)
    ogc = TA.alloc("ogc", [128, 12], F32)
    WB_K = lambda k: [('Wb', k, 0), ('Wb', k, 1)]

    def load_win(l_, pieces, stg):
        for n_, (k, hf) in enumerate(pieces):
            st_, rs_, sm_ = stg[n_ % len(stg)]
            P.dma('sp', sm_, lambda e: e.dma_start(
                out=st_[:, 0:WH], in_=w_in[l_, 128 * k:128 * (k + 1), WH * hf:WH * (hf + 1)]), w=[rs_])
            if hf:
                P.op('act', lambda e: e.activation(
                    out=Wb[:, k, WH * hf:WH * (hf + 1)], in_=st_[:, 0:WH], func=AF.Copy, scale=gcol[:, k:k + 1]),
                    r=[rs_, 'gcol'], w=[('Wb', k, hf)])
            else:
                P.op('dve', lambda e: e.tensor_scalar(
                    out=Wb[:, k, WH * hf:WH * (hf + 1)], in0=st_[:, 0:WH], scalar1=gcol[:, k:k + 1], scalar2=None,
                    op0=ALU.mult), r=[rs_, 'gcol'], w=[('Wb', k, hf)])

    def load_wout(l_, ks):
        for k in ks:
            if k == 0:
                P.dma('sp', 'wgo', lambda e: e.dma_start(out=ogc[:], in_=out_g[l_, :, :]), w=['ogc'])
            P.dma('sp', 'wstC', lambda e: e.dma_start(
                out=wstC[:], in_=w_out[l_, 128 * k:128 * (k + 1), :]), w=['wstC'])
            P.op('dve', lambda e: e.tensor_scalar(
                out=Wo[:, k, :], in0=wstC[:], scalar1=ogc[:, k:k + 1], scalar2=None, op0=ALU.mult),
                r=['wstC', 'ogc'], w=[('Wo', k)])

    ALLW = [(k, hf) for k in range(8) for hf in range(2)]
    ps = nc.alloc_psum_tensor("ps", [128, 4096], F32)
    psb = ps.bitcast(BF16)

    def bank(b, lo=0, hi=512):
        return ps[:, 512 * b + lo: 512 * b + hi]

    def PS(b):
        return ('ps', b)

    P.sec('init')
    P.dma('sp', 'c0', lambda e: e.dma_start(out=ident[:], in_=c_ident[:, :]), w=['ident'])
    P.dma('sp', 'c1', lambda e: e.dma_start(out=tri[:], in_=c_tri[:, :]), w=['tri'])
    P.op('dve', lambda e: e.tensor_copy(out=identb[:], in_=ident[:]), r=['ident'], w=['identb'])
    P.op('dve', lambda e: e.memset(onesf[:], 1.0), w=['onesf'])
    P.op('dve', lambda e: e.memset(epsc[:], EPS), w=['epsc'])
    P.op('dve', lambda e: e.memset(kmT[:], 0.0), w=[('kmT', j) for j in range(4)])
    P.dma('sp', 'c2', lambda e: e.dma_start(out=fq_s[:, 67:70, :], in_=c_ones[:, :, :]), w=[('fq', 'const')])
    P.dma('sp', 'c3', lambda e: e.dma_start(out=fk_s[:, 64:67, :], in_=c_ones[:, :, :]), w=[('fk', 'const')])
    P.dma('sp', 'c4', lambda e: e.dma_start(out=bq_s[:, 80:86, :], in_=c_mq[:, :, :]), w=[('bq', 'const')])
    P.dma('sp', 'c5', lambda e: e.dma_start(out=bk_s[:, 64:86, :], in_=c_mk[:, :, :]), w=[('bk', 'const')])

    for l in range(n_layers):
        x_src = x_in if l == 0 else xr_s
        last = (l == n_layers - 1)
        if 'A' in phases:
            P.barrier()
            A = Arena(nc, wend, HI)
            Wuq = A.alloc("Wuq", [128, 2, 1024], BF16)
            Wukv = A.alloc("Wukv", [128, 1024], BF16)
            gq = A.alloc("gq", [128, 2], F32)
            gkv = A.alloc("gkv", [128, 1], F32)
            bfc = A.alloc("bfc", [8, 1], F32)
            xin_off = A.cur
            xin = [A.alloc("xin0", [128, 4, D], F32)] * 2
            xT = A.alloc("xT", [128, 8, 512], BF16)
            sq = A.alloc("sq", [128, D], F32)
            ssqs = [A.alloc(f"ssq{i}", [128, 4], F32) for i in range(2)]
            rstds = [A.alloc(f"rstd{i}", [128, 4], F32) for i in range(2)]
            Rt = A.alloc("Rt", [128, 4, 128], F32)
            rbc = A.alloc("rbc", [128, 512], F32)
            fmst = [A.alloc(f"fmst{i}", [128, 512], BF16) for i in range(4)]
            qf = A.alloc("qf", [128, 4, 512], F32)
            kf = A.alloc("kf", [128, 512], F32)
            cqT = A.alloc("cqT", [128, 2, 512], F32)
            sq2 = A.alloc("sq2", [128, 2, 512], F32)
            rq = A.alloc("rq", [128, 512], F32)
            cqn = A.alloc("cqn", [128, 2, 512], BF16)
            ckvn = A.alloc("ckvn", [128, 512], BF16)
            cosc = A.alloc("cosc", [128, 512], F32)
            sinc = A.alloc("sinc", [128, 512], F32)
            t1 = A.alloc("t1", [128, 512], F32)
            t2 = A.alloc("t2", [128, 512], F32)
            ffx = A.alloc("ffx", [8, 512], F32)
            ffe = A.alloc("ffe", [8, 512], F32)
            ones8 = A.alloc("ones8", [8, 512], F32)
            cc = [A.alloc(f"cc{i}", [8, 512], F32) for i in range(2)]
            r1 = A.alloc("r1", [8, 512], F32)
            cs = A.alloc("cs", [8, 3, 512], BF16)
            ncs = A.alloc("ncs", [8, 3, 512], BF16)
            vst = A.alloc("vst", [128, 4, 8 * 65], BF16)
            vst2 = A.alloc("vst2", [128, 4, 8 * 65], BF16)
            mvst = A.alloc("mvst", [128, 4, 4 * 129], BF16)
            gzst = A.alloc("gzst", [128, 1536], F32)
            gate = A.alloc("gate", [128, 8, 16], F32)
            mx8 = A.alloc("mx8", [128, 8, 8], F32)
            masks = [A.alloc(f"mask{i}", [128, 8, 16], BF16) for i in range(4)]
            maskT = A.alloc("maskT", [16, 8, 128], BF16)

            P.sec('weights')
            if l == 0:
                wst2 = nc.alloc_sbuf_tensor_at("wst2", [128, WH], F32, offset=xin_off)
                P.dma('sp', 'wg', lambda e: e.dma_start(out=gcol[:], in_=ln_g[l, :, :]), w=['gcol'])
                load_win(0, ALLW, [(wst, 'wst', 'wst'), (wst2, ('xin', 0), 'xin0')])
            WB = [('Wb', k, j) for k in range(8) for j in range(2)]
            P.dma('sp', 'wg2', lambda e: e.dma_start(
                out=gq[:], in_=q_g[l, :, :]), w=['gq'])
            P.dma('sp', 'wg3', lambda e: e.dma_start(
                out=gkv[:], in_=kv_g[l, :, :]), w=['gkv'])
            P.dma('sp', 'wg4', lambda e: e.dma_start(
                out=bfc[:], in_=b_f[l, :, :]), w=['bfc'])
            for k in range(2):
                P.dma('sp', 'wst', lambda e, k=k: e.dma_start(
                    out=wst[:, 0:1024], in_=w_uq[l, 128 * k:128 * (k + 1), :]), w=['wst'])
                P.op('dve', lambda e, k=k: e.tensor_scalar(
                    out=Wuq[:, k, :], in0=wst[:, 0:1024], scalar1=gq[:, k:k + 1], scalar2=None,
                    op0=ALU.mult), r=['wst', 'gq'], w=['Wuq'])
            P.dma('sp', 'wst', lambda e: e.dma_start(
                out=wst[:, 0:1024], in_=w_ukv[l, :, :]), w=['wst'])
            P.op('dve', lambda e: e.tensor_scalar(
                out=Wukv[:], in0=wst[:, 0:1024], scalar1=gkv[:, 0:1], scalar2=None,
                op0=ALU.mult), r=['wst', 'gkv'], w=['Wukv'])
            P.op('dve', lambda e: e.memset(ones8[:], 1.0), w=['ones8'])
            P.op('dve', lambda e: e.memset(cc[1][:], 0.0), w=[('cc', 1)])
            P.op('pool', lambda e: e.memset(vst[:], 1.0), w=['vst'])
            P.op('pool', lambda e: e.memset(vst2[:], 1.0), w=['vst2'])
            P.op('pool', lambda e: e.memset(mvst[:], 1.0), w=['mvst'])

            pb = [0]

            def nb():
                pb[0] = (pb[0] + 1) % 8
                return pb[0]

            def store_pair(st, si, dst, j, tok):
                for hh in range(2):
                    P.dma('sp', f'fm{si}_{hh}', lambda e, hh=hh: e.dma_start(
                        out=dst[2 * j + hh, 0:64, tok], in_=st[64 * hh:64 * hh + 64, :]),
                        r=[('fmst', si)])

            def fm_job(col, ncols, rows_lo=0):
                b = nb()
                for k in range(8):
                    P.op('pe', lambda e, k=k, b=b: e.matmul(
                        bank(b)[rows_lo:rows_lo + ncols, :], lhsT=Wb[:, k, col:col + ncols],
                        rhs=xT[:, k, :], start=(k == 0), stop=(k == 7)),
                        r=WB_K(k) + ['xT'], w=[PS(b)])
                return b

            XI = ('xin', 0)
            xi = xin[0]
            qs = 192.0 ** -0.5

            def tokc(c):
                return slice(512 * c, 512 * (c + 1))

            def load_x(c):
                P.sec('pre')
                P.dma('sp', 'xin0', lambda e: e.dma_start(
                    out=xi[:], in_=x_src[tokc(c), :].rearrange("(t p) d -> p t d", p=128)), w=[XI])

            def load_trig(c):
                P.sec('mla')
                P.dma('sp', 'cosd', lambda e: e.dma_start(out=cosc[:], in_=c_cos[:, tokc(c)]), w=['cosc'])
                P.dma('sp', 'sind', lambda e: e.dma_start(out=sinc[:], in_=c_sin[:, tokc(c)]), w=['sinc'])

            def pre_stats(c):
                P.sec('pre')
                ssq, rstd = ssqs[c % 2], rstds[c % 2]
                SSQ, RSTD = ('ssq', c % 2), ('rstd', c % 2)
                for t in range(4):
                    P.op('dve', lambda e: e.tensor_tensor(
                        out=sq[:], in0=xi[:, t, :], in1=xi[:, t, :], op=ALU.mult), r=[XI], w=['sq'])
                    P.op('dve', lambda e: e.reduce_sum(
                        out=ssq[:, t:t + 1], in_=sq[:], axis=AX.X), r=['sq'], w=[SSQ])
                P.op('act', lambda e: e.activation(
                    out=ssq[:], in_=ssq[:], func=AF.Sqrt, bias=epsc[:, 0:1], scale=1.0 / D),
                    r=[SSQ, 'epsc'], w=[SSQ])
                P.op('dve', lambda e: e.reciprocal(out=rstd[:], in_=ssq[:]), r=[SSQ], w=[RSTD])
                for t in range(4):
                    P.op('dve', lambda e: e.tensor_scalar(
                        out=Rt[:, t, :], in0=onesf[:], scalar1=rstd[:, t:t + 1], scalar2=None, op0=ALU.mult),
                        r=['onesf', RSTD], w=[('Rt', t)], ss=True)

            def pre_pe(c):
                P.sec('pre')
                for k in range(8):
                    b = nb()
                    for t in range(4):
                        P.op('pe', lambda e: e.transpose(
                            out=bank(b, 128 * t, 128 * (t + 1)), in_=xi[:, t, 128 * k:128 * (k + 1)],
                            identity=ident[:]), r=[XI, 'ident'], w=[PS(b)])
                    if k % 2 == 0:
                        P.op('act', lambda e: e.copy(out=xT[:, k, :], in_=bank(b)), r=[PS(b)], w=['xT'])
                    else:
                        P.op('dve', lambda e: e.tensor_copy(out=xT[:, k, :], in_=bank(b)), r=[PS(b)], w=['xT'])
                b = nb()
                for t in range(4):
                    P.op('pe', lambda e: e.matmul(
                        bank(b, 128 * t, 128 * (t + 1)), lhsT=Rt[:, t, :], rhs=ident[:], start=True, stop=True),
                        r=[('Rt', t), 'ident'], w=[PS(b)])
                P.op('act', lambda e: e.copy(out=rbc[:], in_=bank(b)), r=[PS(b)], w=['rbc'])

            def s_fox(c):
                P.sec('fox')
                tok = tokc(c)
                for j in range(4):
                    b = fm_job(C_FQ + 128 * j, 128)
                    st = fmst[j % 4]
                    P.op('dve', lambda e: e.scalar_tensor_tensor(
                        out=st[:], in0=bank(b), scalar=0.125, in1=rbc[:], op0=ALU.mult, op1=ALU.mult),
                        r=[PS(b), 'rbc'], w=[('fmst', j % 4)])
                    store_pair(st, j % 4, fq_s, j, tok)
                for j in range(4):
                    b = fm_job(C_FK + 128 * j, 128)
                    st = fmst[j % 4]
                    P.op('dve', lambda e: e.tensor_tensor(
                        out=st[:], in0=bank(b), in1=rbc[:], op=ALU.mult),
                        r=[PS(b), 'rbc'], w=[('fmst', j % 4)])
                    store_pair(st, j % 4, fk_s, j, tok)

            def s_ff(c):
                P.sec('ff')
                tok = tokc(c)
                b = fm_job(C_FF, 8)
                P.op('dve', lambda e: e.tensor_tensor(
                    out=ffx[:], in0=bank(b)[0:8, :], in1=rbc[0:8, :], op=ALU.mult),
                    r=[PS(b), 'rbc'], w=['ffx'])
                P.op('dve', lambda e: e.tensor_scalar(
                    out=ffx[:], in0=ffx[:], scalar1=bfc[:, 0:1], scalar2=-1.0, op0=ALU.add, op1=ALU.mult),
                    r=['ffx', 'bfc'], w=['ffx'])
                P.op('act', lambda e: e.activation(out=ffe[:], in_=ffx[:], func=AF.Exp), r=['ffx'], w=['ffe'])
                P.op('dve', lambda e: e.tensor_scalar(
                    out=ffe[:], in0=ffe[:], scalar1=1.0, scalar2=None, op0=ALU.add), r=['ffe'], w=['ffe'])
                P.op('act', lambda e: e.activation(out=ffe[:], in_=ffe[:], func=AF.Ln), r=['ffe'], w=['ffe'])
                cprev, ccur = cc[(c + 1) % 2], cc[c % 2]
                if c == 0:
                    P.op('dve', lambda e: e.tensor_tensor_scan(
                        out=ccur[:], data0=ones8[:], data1=ffe[:], initial=0.0,
                        op0=ALU.mult, op1=ALU.subtract), r=['ones8', 'ffe'], w=[('cc', c % 2)])
                else:
                    P.op('dve', lambda e: e.tensor_tensor_scan(
                        out=ccur[:], data0=ones8[:], data1=ffe[:], initial=cprev[:, 511:512],
                        op0=ALU.mult, op1=ALU.subtract),
                        r=['ones8', 'ffe', ('cc', (c + 1) % 2)], w=[('cc', c % 2)], ss=True)
                P.op('dve', lambda e: e.tensor_copy(out=cs[:, 0, :], in_=ccur[:]), r=[('cc', c % 2)], w=['cs'])
                P.op('dve', lambda e: e.tensor_tensor(
                    out=r1[:], in0=ccur[:], in1=cs[:, 0, :], op=ALU.subtract),
                    r=[('cc', c % 2), 'cs'], w=['r1'])
                P.op('dve', lambda e: e.tensor_copy(out=cs[:, 1, :], in_=r1[:]), r=['r1'], w=['cs'])
                P.op('dve', lambda e: e.tensor_tensor(
                    out=r1[:], in0=r1[:], in1=cs[:, 1, :], op=ALU.subtract), r=['r1', 'cs'], w=['r1'])
                P.op('dve', lambda e: e.tensor_copy(out=cs[:, 2, :], in_=r1[:]), r=['r1'], w=['cs'])
                P.op('dve', lambda e: e.tensor_scalar(
                    out=ncs[:], in0=cs[:], scalar1=-1.0, scalar2=None, op0=ALU.mult), r=['cs'], w=['ncs'])
                P.dma('sp', 'csd', lambda e: e.dma_start(out=fq_s[:, 64:67, tok], in_=cs[:]), r=['cs'])
                P.dma('sp', 'ncsd', lambda e: e.dma_start(out=fk_s[:, 67:70, tok], in_=ncs[:]), r=['ncs'])

            def s_moba(c):
                P.sec('moba')
                tok = tokc(c)
                for j in range(4):
                    b = fm_job(C_BQ + 128 * j, 128)
                    st = fmst[j % 4]
                    P.op('dve', lambda e: e.scalar_tensor_tensor(
                        out=qf[:, j, :], in0=bank(b), scalar=0.125, in1=rbc[:], op0=ALU.mult, op1=ALU.mult),
                        r=[PS(b), 'rbc'], w=[('qf', j)])
                    P.op('pool', lambda e: e.tensor_copy(out=st[:], in_=qf[:, j, :]),
                         r=[('qf', j)], w=[('fmst', j % 4)])
                    store_pair(st, j % 4, bq_s, j, tok)
                for j in range(4):
                    b = fm_job(C_BK + 128 * j, 128)
                    st = fmst[j % 4]
                    P.op('dve', lambda e: e.tensor_tensor(
                        out=kf[:], in0=bank(b), in1=rbc[:], op=ALU.mult), r=[PS(b), 'rbc'], w=['kf'])
                    for hh in range(2):
                        P.op('dve', lambda e: e.reduce_sum(
                            out=kmT[64 * hh:64 * hh + 64, j, hh, 2 * c:2 * c + 2],
                            in_=kf[64 * hh:64 * hh + 64, :].rearrange("p (n s) -> p n s", s=256),
                            axis=AX.X), r=['kf'], w=[('kmT', j)])
                    P.op('pool', lambda e: e.tensor_copy(out=st[:], in_=kf[:]),
                         r=['kf'], w=[('fmst', j % 4)])
                    store_pair(st, j % 4, bk_s, j, tok)

            gate_bank = [0]

            def s_gate_mm(c):
                P.sec('gate')
                b = nb()
                gate_bank[0] = b
                for t in range(4):
                    for j in range(4):
                        P.op('pe', lambda e: e.matmul(
                            bank(b, 128 * t + 32 * j, 128 * t + 32 * j + 32),
                            lhsT=qf[:, j, 128 * t:128 * (t + 1)],
                            rhs=kmT[:, j, :, :].rearrange("p a n -> p (a n)"), start=True, stop=True),
                            r=[('qf', j), ('kmT', j)], w=[PS(b)])

            def s_gate_dve(c):
                P.sec('gate_top')
                b = gate_bank[0]
                for t in range(4):
                    cur = 2 * c + t // 2
                    mk = masks[t]
                    MK = ('mask', t)
                    P.op('dve', lambda e: e.memset(gate[:], NEG), w=['gate'])
                    if cur > 0:
                        P.op('dve', lambda e: e.tensor_copy(
                            out=gate[:, :, 0:cur],
                            in_=bank(b, 128 * t, 128 * (t + 1)).rearrange("p (h n) -> p h n", n=16)[:, :, 0:cur]),
                            r=[PS(b)], w=['gate'])
                    for h in range(8):
                        P.op('dve', lambda e: e.max(out=mx8[:, h, :], in_=gate[:, h, :]), r=['gate'], w=['mx8'])
                    for h in range(8):
                        P.op('dve', lambda e: e.tensor_scalar(
                            out=mk[:, h, :], in0=gate[:, h, :], scalar1=mx8[:, h, 2:3], scalar2=NEG,
                            op0=ALU.is_lt, op1=ALU.mult), r=['gate', 'mx8'], w=[MK], ss=True)
                    if cur <= 3 and cur > 0:
                        P.op('dve', lambda e: e.memset(mk[:, :, 0:cur], 0.0), w=[MK])
                    P.op('dve', lambda e: e.memset(mk[:, :, cur:cur + 1], 0.0), w=[MK])
                    if cur < 15:
                        P.op('dve', lambda e: e.memset(mk[:, :, cur + 1:16], NEG), w=[MK])

            def s_gate_tr(c):
                P.sec('gate_tr')
                for t in range(4):
                    mk = masks[t]
                    b2 = nb()
                    for h in range(8):
                        P.op('pe', lambda e: e.transpose(
                            out=psb[0:16, 1024 * b2 + 128 * h: 1024 * b2 + 128 * (h + 1)],
                            in_=mk[:, h, :], identity=identb[:]),
                            r=[('mask', t), 'identb'], w=[PS(b2)])
                    P.op('act', lambda e: e.copy(
                        out=maskT[:],
                        in_=psb[0:16, 1024 * b2:1024 * b2 + 1024].rearrange("p (h q) -> p h q", q=128)),
                        r=[PS(b2)], w=['maskT'])
                    P.dma('sp', 'mskd', lambda e: e.dma_start(
                        out=bq_s[:, 64:80, 512 * c + 128 * t:512 * c + 128 * (t + 1)].rearrange("h n s -> n h s"),
                        in_=maskT[:]), r=['maskT'])

            def s_mla_q_fm(c):
                P.sec('mla')
                for j in range(2):
                    b = fm_job(C_CQ + 128 * j, 128)
                    P.op('dve', lambda e: e.tensor_tensor(
                        out=cqT[:, j, :], in0=bank(b), in1=rbc[:], op=ALU.mult),
                        r=[PS(b), 'rbc'], w=[('cqT', j)])
                    P.op('act', lambda e: e.activation(out=sq2[:, j, :], in_=cqT[:, j, :], func=AF.Square),
                         r=[('cqT', j)], w=[('sq2', j)])

            def s_mla_q_norm(c):
                P.sec('mla')
                b = nb()
                for j in range(2):
                    P.op('pe', lambda e: e.matmul(
                        bank(b), lhsT=onesf[:], rhs=sq2[:, j, :], start=(j == 0), stop=(j == 1)),
                        r=['onesf', ('sq2', j)], w=[PS(b)])
                P.op('act', lambda e: e.activation(
                    out=rq[:], in_=bank(b), func=AF.Sqrt, bias=epsc[:, 0:1], scale=1.0 / 256),
                    r=[PS(b), 'epsc'], w=['rq'])
                P.op('dve', lambda e: e.reciprocal(out=rq[:], in_=rq[:]), r=['rq'], w=['rq'])
                for j in range(2):
                    P.op('dve', lambda e: e.tensor_tensor(
                        out=cqn[:, j, :], in0=cqT[:, j, :], in1=rq[:], op=ALU.mult),
                        r=[('cqT', j), 'rq'], w=['cqn'])

            def s_mla_kv_fm(c):
                P.sec('mla')
                tok = tokc(c)
                b = fm_job(C_CKV, 128)
                P.op('dve', lambda e: e.tensor_tensor(
                    out=cqT[:, 0, :], in0=bank(b), in1=rbc[:], op=ALU.mult), r=[PS(b), 'rbc'], w=[('cqT', 0)])
                P.op('act', lambda e: e.activation(out=sq2[:, 0, :], in_=cqT[:, 0, :], func=AF.Square),
                     r=[('cqT', 0)], w=[('sq2', 0)])
                bA = fm_job(C_KR, 64)
                bB = fm_job(C_KRS, 64)
                P.op('dve', lambda e: e.tensor_tensor(
                    out=t1[0:64, :], in0=bank(bA)[0:64, :], in1=rbc[0:64, :], op=ALU.mult),
                    r=[PS(bA), 'rbc'], w=['t1'])
                P.op('dve', lambda e: e.tensor_tensor(
                    out=t2[0:64, :], in0=bank(bB)[0:64, :], in1=rbc[0:64, :], op=ALU.mult),
                    r=[PS(bB), 'rbc'], w=['t2'])
                P.op('pool', lambda e: e.tensor_tensor(
                    out=t1[0:64, :], in0=t1[0:64, :], in1=cosc[0:64, :], op=ALU.mult),
                    r=['t1', 'cosc'], w=['t1'])
                P.op('pool', lambda e: e.tensor_tensor(
                    out=t2[0:64, :], in0=t2[0:64, :], in1=sinc[0:64, :], op=ALU.mult),
                    r=['t2', 'sinc'], w=['t2'])
                st = fmst[2]
                P.op('pool', lambda e: e.tensor_tensor(
                    out=st[0:64, :], in0=t1[0:64, :], in1=t2[0:64, :], op=ALU.add),
                    r=['t1', 't2'], w=[('fmst', 2)])
                P.dma('sp', 'fm2', lambda e: e.dma_start(
                    out=mk2_s[:, tok], in_=st[0:64, :]), r=[('fmst', 2)])

            def s_mla_kv_norm(c):
                P.sec('mla')
                b = nb()
                P.op('pe', lambda e: e.matmul(
                    bank(b), lhsT=onesf[:], rhs=sq2[:, 0, :], start=True, stop=True),
                    r=['onesf', ('sq2', 0)], w=[PS(b)])
                P.op('act', lambda e: e.activation(
                    out=rq[:], in_=bank(b), func=AF.Sqrt, bias=epsc[:, 0:1], scale=1.0 / 128),
                    r=[PS(b), 'epsc'], w=['rq'])
                P.op('dve', lambda e: e.reciprocal(out=rq[:], in_=rq[:]), r=['rq'], w=['rq'])
                P.op('dve', lambda e: e.tensor_tensor(
                    out=ckvn[:], in0=cqT[:, 0, :], in1=rq[:], op=ALU.mult), r=[('cqT', 0), 'rq'], w=['ckvn'])

            def s_mla_q_up(c):
                P.sec('mla')
                tok = tokc(c)
                for h in range(4):
                    b = nb()
                    for k in range(2):
                        P.op('pe', lambda e: e.matmul(
                            bank(b), lhsT=Wuq[:, k, 128 * h:128 * (h + 1)], rhs=cqn[:, k, :],
                            start=(k == 0), stop=(k == 1)), r=['Wuq', 'cqn'], w=[PS(b)])
                    st = fmst[h % 4]
                    P.op('act', lambda e: e.mul(out=st[:], in_=bank(b), mul=qs),
                         r=[PS(b)], w=[('fmst', h % 4)])
                    P.dma('sp', f'fm{h % 4}', lambda e: e.dma_start(
                        out=mq1_s[h, :, tok], in_=st[:]), r=[('fmst', h % 4)])
                for p in range(2):
                    bA, bB = nb(), nb()
                    for k in range(2):
                        P.op('pe', lambda e: e.matmul(
                            bank(bA), lhsT=Wuq[:, k, 512 + 128 * p:512 + 128 * (p + 1)], rhs=cqn[:, k, :],
                            start=(k == 0), stop=(k == 1)), r=['Wuq', 'cqn'], w=[PS(bA)])
                    for k in range(2):
                        P.op('pe', lambda e: e.matmul(
                            bank(bB), lhsT=Wuq[:, k, 768 + 128 * p:768 + 128 * (p + 1)], rhs=cqn[:, k, :],
                            start=(k == 0), stop=(k == 1)), r=['Wuq', 'cqn'], w=[PS(bB)])
                    P.op('dve', lambda e: e.tensor_tensor(
                        out=t1[:], in0=bank(bA), in1=cosc[:], op=ALU.mult), r=[PS(bA), 'cosc'], w=['t1'])
                    P.op('dve', lambda e: e.tensor_tensor(
                        out=t2[:], in0=bank(bB), in1=sinc[:], op=ALU.mult), r=[PS(bB), 'sinc'], w=['t2'])
                    P.op('pool', lambda e: e.tensor_tensor(
                        out=t1[:], in0=t1[:], in1=t2[:], op=ALU.add), r=['t1', 't2'], w=['t1'])
                    st = fmst[p]
                    P.op('act', lambda e: e.mul(out=st[:], in_=t1[:], mul=qs), r=['t1'], w=[('fmst', p)])
                    store_pair(st, p, mq2_s, p, tok)

            def s_mla_kv_up(c):
                P.sec('mla')
                tok = tokc(c)
                for h in range(4):
                    b = nb()
                    P.op('pe', lambda e: e.matmul(
                        bank(b), lhsT=Wukv[:, 128 * h:128 * (h + 1)], rhs=ckvn[:], start=True, stop=True),
                        r=['Wukv', 'ckvn'], w=[PS(b)])
                    st = fmst[h % 4]
                    P.op('act', lambda e: e.copy(out=st[:], in_=bank(b)), r=[PS(b)], w=[('fmst', h % 4)])
                    P.dma('sp', f'fm{h % 4}', lambda e: e.dma_start(
                        out=mk1_s[h, :, tok], in_=st[:]), r=[('fmst', h % 4)])
                for t in range(4):
                    b = nb()
                    P.op('pe', lambda e: e.matmul(
                        bank(b), lhsT=ckvn[:, 128 * t:128 * (t + 1)], rhs=Wukv[:, 512:1024],
                        start=True, stop=True), r=['Wukv', 'ckvn'], w=[PS(b)])
                    P.op('act', lambda e: e.copy(
                        out=mvst[:, t, :].rearrange("p (h d) -> p h d", d=129)[:, :, 0:128],
                        in_=bank(b).rearrange("p (h d) -> p h d", d=128)), r=[PS(b)], w=['mvst'])
                P.dma('sp', 'mvd', lambda e: e.dma_start(
                    out=mv_s[tok, :].rearrange("(t p) f -> p t f", p=128), in_=mvst[:]), r=['mvst'])

            def s_tm(c, tiles):
                P.sec('tm')
                for t in tiles:
                    for (col, kind) in ((C_FV, 'fv'), (C_BV, 'bv'), (C_FZ, 'z0'), (C_MZ, 'z1'), (C_BZ, 'z2')):
                        b = nb()
                        for k in range(8):
                            P.op('pe', lambda e: e.matmul(
                                bank(b), lhsT=xT[:, k, 128 * t:128 * (t + 1)], rhs=Wb[:, k, col:col + 512],
                                start=(k == 0), stop=(k == 7)), r=WB_K(k) + ['xT'], w=[PS(b)])
                        if kind in ('fv', 'bv'):
                            vs = vst if kind == 'fv' else vst2
                            P.op('act', lambda e: e.activation(
                                out=vs[:, t, :].rearrange("p (h d) -> p h d", d=65)[:, :, 0:64],
                                in_=bank(b).rearrange("p (h d) -> p h d", d=64), func=AF.Copy,
                                scale=rstds[c % 2][:, t:t + 1]), r=[PS(b), ('rstd', c % 2)],
                                w=['vst' if kind == 'fv' else 'vst2'])
                        else:
                            g = int(kind[1])
                            P.op('act', lambda e: e.activation(
                                out=gzst[:, 512 * g:512 * (g + 1)], in_=bank(b), func=AF.Silu,
                                scale=rstds[c % 2][:, t:t + 1]), r=[PS(b), ('rstd', c % 2)], w=['gzst'])
                    P.dma('sp', 'gzd', lambda e: e.dma_start(
                        out=gz_s[512 * c + 128 * t:512 * c + 128 * (t + 1), :], in_=gzst[:]), r=['gzst'])

            def s_vstore(c):
                P.sec('tm')
                tok = tokc(c)
                P.dma('sp', 'fvd', lambda e: e.dma_start(
                    out=fv_s[tok, :].rearrange("(t p) f -> p t f", p=128), in_=vst[:]), r=['vst'])
                P.dma('sp', 'bvd', lambda e: e.dma_start(
                    out=bv_s[tok, :].rearrange("(t p) f -> p t f", p=128), in_=vst2[:]), r=['vst2'])

            NCR = NCH_RUN[0]
            load_x(0)
            pre_stats(0)
            pre_pe(0)
            for c in range(NCR):
                if c + 1 < NCR:
                    load_x(c + 1)
                load_trig(c)
                s_fox(c)
                s_ff(c)
                s_moba(c)
                s_mla_q_fm(c)
                s_tm(c, (0,))
                s_gate_mm(c)
                s_gate_dve(c)
                s_mla_q_norm(c)
                if c + 1 < NCR:
                    pre_stats(c + 1)
                s_tm(c, (1,))
                s_mla_kv_fm(c)
                s_tm(c, (2,))
                s_mla_kv_norm(c)
                s_tm(c, (3,))
                s_vstore(c)
                if c + 1 < NCR:
                    pre_pe(c + 1)
                s_mla_q_up(c)
                s_mla_kv_up(c)
                s_gate_tr(c)

        if 'B' in phases:
            mixers = (
                dict(name='fox', H=8, dv=64, rows=[70], q=[fq_s], k=[fk_s], v=fv_s, g=0,
                     qres=lambda c: [('fq', 'const'), ('fq', c, 'c')] + [('fq', c, j) for j in range(4)],
                     kres=lambda c: [('fk', 'const'), ('fk', c, 'c')] + [('fk', c, j) for j in range(4)],
                     vres=lambda c: [('fv', c)]),
                dict(name='mla', H=4, dv=128, rows=[128, 64], q=[mq1_s, mq2_s], k=[mk1_s, mk2_s], v=mv_s, g=1,
                     qres=lambda c: [('mq1', c, h) for h in range(4)] + [('mq2', c, p) for p in range(2)],
                     kres=lambda c: [('mk1', c, h) for h in range(4)] + [('mk2', c)],
                     vres=lambda c: [('mv', c)]),
                dict(name='moba', H=8, dv=64, rows=[86], q=[bq_s], k=[bk_s], v=bv_s, g=2,
                     qres=lambda c: [('bq', 'const'), ('bq', c, 'm')] + [('bq', c, j) for j in range(4)],
                     kres=lambda c: [('bk', 'const')] + [('bk', c, j) for j in range(4)],
                     vres=lambda c: [('bv', c)]),
            )
            for mx in mixers:
                P.barrier()
                B = Arena(nc, persist_end, HI)
                H, dv, rows = mx['H'], mx['dv'], mx['rows']
                dv1 = dv + 1
                np_ = len(rows)
                nm = mx['name']
                G = mx['g']
                GB = 3 if 4 * dv1 <= 512 else 2
                kT = [B.alloc(f"kT{i}", [rows[i], H if not (nm == 'mla' and i == 1) else 1, S], BF16)
                      for i in range(np_)]
                vv = B.alloc("vv", [128, NT, H * dv1], BF16)
                qT = [[B.alloc(f"qT{i}_{bf}", [rows[i], H, 512], BF16) for i in range(np_)] for bf in range(2)]
                pT = [B.alloc(f"pT{i}", [128, GB * 512], BF16) for i in range(3)]
                oo = B.alloc("oo", [128, 4, 512], F32)
                gzt = [B.alloc(f"gzt{i}", [128, 4, 512], F32) for i in range(3)]
                rcp = B.alloc("rcp", [128, 4], F32)
                osq = B.alloc("osq", [128, 512], F32)
                oss = B.alloc("oss", [128, 4], F32)
                ors = B.alloc("ors", [128, 4], F32)
                yst = B.alloc("yst", [128, 4, 512], BF16)

                if GB == 3:
                    def acc(a_, u):
                        base = 512 * (6 + a_)
                        return ps[:, base + dv1 * u: base + dv1 * (u + 1)]

                    def ACC(a_):
                        return [('ps', 6 + a_)]

                    def acc_all(a_):
                        return ps[:, 512 * (6 + a_): 512 * (6 + a_) + 4 * dv1]
                else:
                    def acc(a_, u):
                        base = 2048 + 1024 * a_ + 512 * (u // 2) + dv1 * (u % 2)
                        return ps[:, base: base + dv1]

                    def ACC(a_):
                        return [('ps', 4 + 2 * a_), ('ps', 5 + 2 * a_)]

                    def acc_all(a_):
                        return ps[:, 2048 + 1024 * a_: 2048 + 1024 * a_ + 512 + 2 * dv1]

                def loads(c):
                    tok = slice(512 * c, 512 * (c + 1))
                    qb = qT[c % 2]
                    for i in range(np_):
                        if nm == 'mla' and i == 1:
                            P.dma('sp', f'k{i}', lambda e: e.dma_start(
                                out=kT[i][:, 0, tok], in_=mx['k'][i][:, tok]), w=[(nm + 'kT', i, c)])
                        else:
                            P.dma('sp', f'k{i}', lambda e: e.dma_start(
                                out=kT[i][:, :, tok], in_=mx['k'][i][:, :, tok].rearrange("h r s -> r h s")),
                                w=[(nm + 'kT', i, c)])
                        P.dma('sp', f'q{i}{c % 2}', lambda e: e.dma_start(
                            out=qb[i][:], in_=mx['q'][i][:, :, tok].rearrange("h r s -> r h s")),
                            w=[(nm + 'qT', c % 2, i)])
                    P.dma('sp', 'vl', lambda e: e.dma_start(
                        out=vv[:, 4 * c:4 * c + 4, :], in_=mx['v'][tok, :].rearrange("(t p) f -> p t f", p=128)),
                        w=[(nm + 'vv', c)])
                    P.dma('sp', f'gl{c % 3}', lambda e: e.dma_start(
                        out=gzt[c % 3][:], in_=gz_s[tok, 512 * G:512 * (G + 1)].rearrange("(t p) f -> p t f", p=128)),
                        w=[('gzt', c % 3)])

                groups = []
                hc = 0
                for c in range(NCH):
                    for h in range(H):
                        full = [(kt, 0, 0) for kt in range(4 * c)]
                        gl = [[(kt, 0, 512 * i) for i, (kt, _, _) in enumerate(full[i0:i0 + GB])]
                              for i0 in range(0, len(full), GB)]
                        if GB == 3:
                            gl.append([(4 * c + 0, 0, 0), (4 * c + 1, 1, 512), (4 * c + 3, 3, 896), (4 * c + 2, 2, 1024)])
                        else:
                            gl.append([(4 * c + 0, 0, 0), (4 * c + 1, 1, 512)])
                            gl.append([(4 * c + 2, 2, 0), (4 * c + 3, 3, 256)])
                        for gi, g in enumerate(gl):
                            groups.append(dict(c=c, h=h, tiles=g, fh=(gi == 0), lh=(gi == len(gl) - 1), aset=hc % 2))
                        hc += 1

                def emit_score(g, slot):
                    c, h = g['c'], g['h']
                    qb = qT[c % 2]
                    if g['fh']:
                        P.op('dve', lambda e: e.memset(acc_all(g['aset']), 0.0), w=ACC(g['aset']))
                    for ti, (kt, u0, off) in enumerate(g['tiles']):
                        b = slot * GB + off // 512
                        lo = 512 * slot * GB + off
                        wd = 512 - 128 * u0
                        diag = kt >= 4 * c
                        for i in range(np_):
                            hk = 0 if (nm == 'mla' and i == 1) else h
                            P.op('pe', lambda e: e.matmul(
                                ps[:, lo:lo + wd], lhsT=kT[i][:, hk, 128 * kt:128 * (kt + 1)],
                                rhs=qb[i][:, h, 128 * u0:512], start=(i == 0),
                                stop=(i == np_ - 1 and not diag), skip_group_check=diag),
                                r=[(nm + 'kT', i, kt // 4), (nm + 'qT', c % 2, i)], w=[PS(b)])
                        if diag:
                            P.op('pe', lambda e: e.matmul(
                                ps[:, lo:lo + 128], lhsT=identb[:], rhs=tri[:],
                                start=False, stop=True, skip_group_check=True),
                                r=['identb', 'tri'], w=[PS(b)])

                def emit_exp(g, slot, pb_):
                    tl = g['tiles']
                    wtot = max(off + 512 - 128 * u0 for (_, u0, off) in tl)
                    lo = 512 * slot * GB
                    banks = sorted(set(slot * GB + off // 512 for (_, _, off) in tl))
                    P.op('act', lambda e: e.activation(
                        out=pT[pb_][:, 0:wtot], in_=ps[:, lo:lo + wtot], func=AF.Exp),
                        r=[PS(b_) for b_ in banks], w=[('pT', pb_)])

                def emit_pv(g, pb_):
                    h, a_ = g['h'], g['aset']
                    for ti, (kt, u0, off) in enumerate(g['tiles']):
                        for u in range(u0, 4):
                            P.op('pe', lambda e: e.matmul(
                                acc(a_, u), lhsT=pT[pb_][:, off + 128 * (u - u0):off + 128 * (u - u0 + 1)],
                                rhs=vv[:, kt, dv1 * h:dv1 * (h + 1)], start=False, stop=False,
                                skip_group_check=True),
                                r=[('pT', pb_), (nm + 'vv', kt // 4)], w=ACC(a_))

                def head_epilogue(g):
                    h, a_ = g['h'], g['aset']
                    for u in range(4):
                        P.op('dve', lambda e: e.reciprocal(
                            out=rcp[:, u:u + 1], in_=acc(a_, u)[:, dv:dv1]), r=ACC(a_), w=['rcp'])
                    for u in range(4):
                        P.op('dve', lambda e: e.tensor_scalar(
                            out=oo[:, u, dv * h:dv * (h + 1)], in0=acc(a_, u)[:, 0:dv],
                            scalar1=rcp[:, u:u + 1], scalar2=None, op0=ALU.mult),
                            r=ACC(a_) + ['rcp'], w=['oo'], ss=True)

                def chunk_epilogue_a(c):
                    for u in range(4):
                        P.op('dve', lambda e: e.tensor_tensor(
                            out=osq[:], in0=oo[:, u, :], in1=oo[:, u, :], op=ALU.mult), r=['oo'], w=['osq'])
                        P.op('dve', lambda e: e.reduce_sum(out=oss[:, u:u + 1], in_=osq[:], axis=AX.X),
                             r=['osq'], w=['oss'])

                def chunk_epilogue_b(c):
                    tok = slice(512 * c, 512 * (c + 1))
                    P.op('act', lambda e: e.activation(
                        out=oss[:], in_=oss[:], func=AF.Ln, bias=epsc[:, 0:1], scale=1.0 / 512),
                        r=['oss', 'epsc'], w=['oss'])
                    P.op('act', lambda e: e.activation(out=ors[:], in_=oss[:], func=AF.Exp, scale=-0.5),
                         r=['oss'], w=['ors'], ss=True)
                    for u in range(4):
                        P.op('dve', lambda e: e.scalar_tensor_tensor(
                            out=yst[:, u, :], in0=oo[:, u, :], scalar=ors[:, u:u + 1], in1=gzt[c % 3][:, u, :],
                            op0=ALU.mult, op1=ALU.mult), r=['oo', 'ors', ('gzt', c % 3)], w=['yst'], ss=True)
                    P.dma('pool', 'yd', lambda e: e.dma_start(
                        out=yy_s[tok, 512 * G:512 * (G + 1)].rearrange("(t p) f -> p t f", p=128), in_=yst[:]),
                        r=['yst'])

                loads(0)
                emit_score(groups[0], 0)
                ng = len(groups)
                pend = []
                for gi, g in enumerate(groups):
                    if pend and pend[0][0] <= gi:
                        chunk_epilogue_b(pend.pop(0)[1])
                    if g['fh'] and g['h'] == 0 and g['c'] + 1 < NCH:
                        loads(g['c'] + 1)
                    if g['fh'] and g['h'] == 0 and nm == 'moba' and 'C' in phases and g['c'] < 6:
                        load_wout(l, range(2 * g['c'], 2 * g['c'] + 2))
                    emit_exp(g, gi % 2, gi % 3)
                    if gi + 1 < ng:
                        emit_score(groups[gi + 1], (gi + 1) % 2)
                    emit_pv(g, gi % 3)
                    if g['lh']:
                        head_epilogue(g)
                        if g['h'] == H - 1:
                            chunk_epilogue_a(g['c'])
                            pend.append((gi + 3, g['c']))
                for _, c_ in pend:
                    chunk_epilogue_b(c_)

        if 'C' in phases:
            P.barrier()
            Cc = Arena(nc, wend, TOPLO)
            wstn = Cc.alloc("wstn", [128, WH], F32)
            yin = Cc.alloc("yin", [128, 4, 1536], BF16)
            yT = Cc.alloc("yT", [128, 12, 512], BF16)
            xin = Cc.alloc("xinC", [128, 4, D], F32)
            x1 = Cc.alloc("x1", [128, 4, D], F32)
            fg = Cc.alloc("fg", [128, D], F32)
            sq = Cc.alloc("sqC", [128, D], F32)
            ssq = Cc.alloc("ssqC", [128, 4], F32)
            rstd = Cc.alloc("rstdC", [128, 4], F32)
            if 'B' not in phases:
                load_wout(l, range(12))
            if not last:
                P.dma('sp', 'wg', lambda e: e.dma_start(out=gcol[:], in_=ln_g[l + 1, :, :]), w=['gcol'])
            WO = [('Wo', k) for k in range(12)]
            if last:
                P.dma('sp', 'wg2', lambda e: e.dma_start(
                    out=fg[:], in_=fin_g.ap().partition_broadcast(128)), w=['fg'])
            pb = [0]
            for c in range(NCH):
                tok = slice(512 * c, 512 * (c + 1))
                if not last:
                    load_win(l + 1, ALLW[2 * c:2 * c + 2], [(wst, 'wst', 'wst'), (wstn, 'wstn', 'wstn')])
                P.dma('sp', 'yl', lambda e, tok=tok: e.dma_start(
                    out=yin[:], in_=yy_s[tok, :].rearrange("(t p) f -> p t f", p=128)),
                    r=[('yy', c, g) for g in range(3)], w=['yin'])
                P.dma('sp', 'xl', lambda e, tok=tok: e.dma_start(
                    out=xin[:], in_=x_src[tok, :].rearrange("(t p) d -> p t d", p=128)), w=['xinC'])
                for k in range(12):
                    b = pb[0] % 4
                    pb[0] += 1
                    for t in range(4):
                        P.op('pe', lambda e, k=k, t=t, b=b: e.transpose(
                            out=psb[:, 1024 * b + 128 * t:1024 * b + 128 * (t + 1)],
                            in_=yin[:, t, 128 * k:128 * (k + 1)], identity=identb[:]),
                            r=['yin', 'identb'], w=[PS(b)])
                    P.op('act' if k % 2 else 'dve',
                         (lambda e, k=k, b=b: e.copy(out=yT[:, k, :], in_=psb[:, 1024 * b:1024 * b + 512])) if k % 2 else
                         (lambda e, k=k, b=b: e.tensor_copy(out=yT[:, k, :], in_=psb[:, 1024 * b:1024 * b + 512])),
                         r=[PS(b)], w=['yT'])
                for t in range(4):
                    for hf in range(2):
                        b = 4 + pb[0] % 4
                        pb[0] += 1
                        for k in range(12):
                            P.op('pe', lambda e, k=k, t=t, hf=hf, b=b: e.matmul(
                                bank(b), lhsT=yT[:, k, 128 * t:128 * (t + 1)], rhs=Wo[:, k, 512 * hf:512 * (hf + 1)],
                                start=(k == 0), stop=(k == 11)), r=WO + ['yT'], w=[PS(b)])
                        P.op('dve', lambda e, t=t, hf=hf, b=b: e.tensor_tensor(
                            out=x1[:, t, 512 * hf:512 * (hf + 1)], in0=bank(b), in1=xin[:, t, 512 * hf:512 * (hf + 1)],
                            op=ALU.add), r=[PS(b), 'xinC'], w=['x1'])
                if not last:
                    P.dma('pool', 'xs', lambda e, tok=tok: e.dma_start(
                        out=xr_s[tok, :].rearrange("(t p) d -> p t d", p=128), in_=x1[:]),
                        r=['x1'], w=[('xr', c)])
                else:
                    for t in range(4):
                        P.op('act', lambda e, t=t: e.activation(out=sq[:], in_=x1[:, t, :], func=AF.Square),
                             r=['x1'], w=['sqC'])
                        P.op('dve', lambda e, t=t: e.reduce_sum(out=ssq[:, t:t + 1], in_=sq[:], axis=AX.X),
                             r=['sqC'], w=['ssqC'])
                    P.op('act', lambda e: e.activation(
                        out=ssq[:], in_=ssq[:], func=AF.Sqrt, bias=epsc[:, 0:1], scale=1.0 / D),
                        r=['ssqC', 'epsc'], w=['ssqC'])
                    P.op('dve', lambda e: e.reciprocal(out=rstd[:], in_=ssq[:]), r=['ssqC'], w=['rstdC'])
                    for t in range(4):
                        P.op('dve', lambda e, t=t: e.scalar_tensor_tensor(
                            out=x1[:, t, :], in0=x1[:, t, :], scalar=rstd[:, t:t + 1], in1=fg[:],
                            op0=ALU.mult, op1=ALU.mult), r=['x1', 'rstdC', 'fg'], w=['x1'], ss=True)
                    P.dma('pool', 'xs', lambda e, tok=tok: e.dma_start(
                        out=y_out[tok, :].rearrange("(t p) d -> p t d", p=128), in_=x1[:]),
                        r=['x1'], w=[('yout', c)])
    P.build()
    return nc


def host_constants():
    bf = ml_dtypes.bfloat16
    f32 = np.float32
    pos = np.arange(S, dtype=f32)
    inv = (f32(10000.0) ** (-np.arange(0, 64, 2, dtype=f32) / f32(64))).astype(f32)
    ang = (pos[:, None] * inv[None, :]).astype(f32)
    cos = np.cos(ang).astype(f32).T
    sin = np.sin(ang).astype(f32).T
    cos64 = np.concatenate([cos, cos], 0)
    sin64 = np.concatenate([-sin, sin], 0)
    c_cos = np.ascontiguousarray(np.concatenate([cos64, cos64], 0))
    c_sin = np.ascontiguousarray(np.concatenate([sin64, sin64], 0))
    kk = np.arange(128)[:, None]
    qq = np.arange(128)[None, :]
    tri = np.where(qq >= kk, 0.0, NEG).astype(bf)

    def split3(v):
        v = v.astype(f32)
        a = v.astype(bf)
        r = (v - a.astype(f32)).astype(f32)
        b = r.astype(bf)
        r2 = (r - b.astype(f32)).astype(f32)
        return np.stack([a, b, r2.astype(bf)], axis=-2)

    slopes = (f32(2.0) ** (-f32(8.0) * np.arange(1, 9, dtype=f32) / f32(8))).astype(f32)
    mp = (slopes[:, None] * pos[None, :]).astype(f32)
    ones = np.ones((8, 3, S), dtype=bf)
    c_mq = np.concatenate([split3(-mp), ones], axis=1)
    onehot = (np.arange(16)[:, None] == (np.arange(S)[None, :] // 256)).astype(f32)
    c_mk = np.concatenate([np.broadcast_to(onehot[None].astype(bf), (8, 16, S)), ones, split3(mp)], axis=1)
    return dict(c_ident=np.eye(128, dtype=f32), c_tri=tri, c_cos=c_cos, c_sin=c_sin,
                c_ones=ones, c_mq=np.ascontiguousarray(c_mq.astype(bf)),
                c_mk=np.ascontiguousarray(c_mk.astype(bf)))


def host_layout(inputs):
    f32 = np.float32
    w_in = np.asarray(inputs["w_in"], dtype=f32)
    kr = w_in[:, :, C_KR:C_KR + 64]
    krs = np.concatenate([kr[:, :, 32:], kr[:, :, :32]], axis=-1)
    w_in_x = np.ascontiguousarray(np.concatenate([w_in, krs], axis=-1))
    w_uq = np.asarray(inputs["mla_w_uq"], dtype=f32).reshape(2, 256, 4, 192)
    nope = w_uq[..., :128].reshape(2, 256, 512)
    rope = w_uq[..., 128:]
    ropes = np.concatenate([rope[..., 32:], rope[..., :32]], axis=-1)
    w_uq_x = np.ascontiguousarray(np.concatenate(
        [nope, rope.reshape(2, 256, 256), ropes.reshape(2, 256, 256)], axis=-1))
    w_ukv = np.asarray(inputs["mla_w_ukv"], dtype=f32).reshape(2, 128, 4, 256)
    w_ukv_x = np.ascontiguousarray(np.concatenate(
        [w_ukv[..., :128].reshape(2, 128, 512), w_ukv[..., 128:].reshape(2, 128, 512)], axis=-1))
    common = dict(
        ln_g=np.ascontiguousarray(np.asarray(inputs["ln_g"], f32).reshape(2, 8, 128).transpose(0, 2, 1)),
        w_in=w_in_x,
        fox_b_f=np.asarray(inputs["fox_b_f"], f32).reshape(2, 8, 1),
        mla_q_g=np.ascontiguousarray(np.asarray(inputs["mla_q_g"], f32).reshape(2, 2, 128).transpose(0, 2, 1)),
        mla_w_uq=w_uq_x, mla_kv_g=np.asarray(inputs["mla_kv_g"], f32).reshape(2, 128, 1),
        mla_w_ukv=w_ukv_x,
        out_g=np.ascontiguousarray(np.asarray(inputs["out_g"], f32).reshape(2, 12, 128).transpose(0, 2, 1)),
        w_out=np.asarray(inputs["w_out"], f32),
        final_g=np.asarray(inputs["final_g"], f32))
    common.update(host_constants())
    return common


_NC_CACHE = {}


def kernel(**inputs):
    x = np.asarray(inputs["x"], dtype=np.float32)
    common = host_layout(inputs)
    if 'nc' not in _NC_CACHE:
        _NC_CACHE['nc'] = build_program()
    nc = _NC_CACHE['nc']
    in_maps = [dict(common, x=np.ascontiguousarray(x[b])) for b in range(8)]
    res = run_bass_kernel_spmd(nc, in_maps, core_ids=list(range(8)))
    return np.stack([np.asarray(r["y"], dtype=np.float32) for r in res.results], axis=0)
```

```python
import numpy as np
import ml_dtypes
import concourse.bass as bass
import concourse.mybir as mybir
from concourse.bass_utils import run_bass_kernel_spmd

F32 = mybir.dt.float32
BF16 = mybir.dt.bfloat16
AF = mybir.ActivationFunctionType
ALU = mybir.AluOpType
AX = mybir.AxisListType

S = 4096
D = 1024
NCH = 8
NT = 32
DIN = 5064
DINX = 5128
EPS = 1e-6
NEG = -30000.0
C_FQ, C_FK, C_FV, C_FF, C_FZ = 0, 512, 1024, 1536, 1544
C_CQ, C_CKV, C_KR, C_MZ = 2056, 2312, 2440, 2504
C_BQ, C_BK, C_BV, C_BZ = 3016, 3528, 4040, 4552
C_KRS = 5064

ENGS = ('pe', 'act', 'dve', 'pool', 'sp')
SKIP = set()
NCH_RUN = [8]
SAME_ENG_SYNC = True


class _Rec:
    def __init__(self):
        self.call = None

    def __getattr__(self, name):
        def f(*a, **kw):
            self.call = (name, a, kw)
            return self
        return f


def _capture(fn):
    r = _Rec()
    fn(r)
    name, a, kw = r.call
    return lambda eng: getattr(eng, name)(*a, **kw)


class Prog:
    def __init__(self, nc):
        self.nc = nc
        self.ops = {e: [] for e in ENGS}
        self.last_w = {}
        self.readers = {}
        self.dma_n = {}
        self.base = set()
        self.skip = False

    def _deps(self, r, w):
        d = set(self.base)
        for x in r:
            ev = self.last_w.get(x)
            if ev is not None:
                d.add(ev)
        for x in w:
            ev = self.last_w.get(x)
            if ev is not None:
                d.add(ev)
            for ev in self.readers.get(x, {}).values():
                d.add(ev)
        return d

    def _commit(self, ev, r, w):
        for x in r:
            self.readers.setdefault(x, {})[ev[:2]] = ev
        for x in w:
            self.last_w[x] = ev
            self.readers[x] = {}

    def sec(self, name):
        self.skip = name in SKIP

    def op(self, eng, fn, r=(), w=(), ss=False):
        if self.skip:
            return
        idx = len(self.ops[eng])
        ev = ('e', eng, idx)
        deps = self._deps(r, w)
        self.ops[eng].append(dict(fn=_capture(fn), deps=deps, dma=None, ss=ss))
        self._commit(ev, r, w)

    def dma(self, eng, sem, fn, r=(), w=()):
        if self.skip:
            return
        n = self.dma_n.get(sem, 0) + 1
        self.dma_n[sem] = n
        ev = ('d', sem, n)
        deps = self._deps(r, w)
        if n > 1:
            deps.add(('d', sem, n - 1))
        self.ops[eng].append(dict(fn=_capture(fn), deps=deps, dma=sem))
        self._commit(ev, r, w)

    def barrier(self):
        b = set()
        for e in ENGS:
            for idx in range(len(self.ops[e]) - 1, -1, -1):
                if self.ops[e][idx]['dma'] is None:
                    b.add(('e', e, idx))
                    break
        for sem, n in self.dma_n.items():
            b.add(('d', sem, n))
        self.base = b

    def build(self):
        nc = self.nc
        ops = self.ops
        dma_n = self.dma_n
        plan = {e: [] for e in ENGS}
        sig = {e: set() for e in ENGS}
        for e in ENGS:
            waited = {}
            for o in ops[e]:
                need = {}
                for (kind, key, val) in o['deps']:
                    if kind == 'e' and key == e and (e == 'pe' or not (SAME_ENG_SYNC or o.get('ss'))):
                        continue
                    k2 = (kind, key)
                    if waited.get(k2, -1) >= val:
                        continue
                    need[k2] = max(need.get(k2, -1), val)
                for k2, v in need.items():
                    waited[k2] = v
                    if k2[0] == 'e':
                        sig[k2[1]].add(v)
                plan[e].append(need)
        cnt = {e: {idx: i + 1 for i, idx in enumerate(sorted(sig[e]))} for e in ENGS}
        sems = {e: nc.alloc_semaphore('s_' + e) for e in ENGS}
        dsems = {k: nc.alloc_semaphore('d_' + k) for k in self.dma_n}

        def replay(e, eng):
            lastd = {}
            for idx, o in enumerate(ops[e]):
                for k2, v in plan[e][idx].items():
                    if k2[0] == 'e':
                        eng.wait_ge(sems[k2[1]], cnt[k2[1]][v])
                    else:
                        eng.wait_ge(dsems[k2[1]], 16 * v)
                        lastd[k2[1]] = v
                ins = o['fn'](eng)
                if o['dma'] is not None:
                    ins.then_inc(dsems[o['dma']], 16)
                elif idx in cnt[e]:
                    ins.then_inc(sems[e], 1)
            if e == 'sp':
                for k, n in dma_n.items():
                    if lastd.get(k, 0) < n:
                        eng.wait_ge(dsems[k], 16 * n)

        with nc.Block() as block:
            @block.tensor
            def _(eng):
                replay('pe', eng)

            @block.scalar
            def _(eng):
                replay('act', eng)

            @block.vector
            def _(eng):
                replay('dve', eng)

            @block.gpsimd
            def _(eng):
                replay('pool', eng)

            @block.sync
            def _(eng):
                replay('sp', eng)


class Arena:
    def __init__(self, nc, lo, hi):
        self.nc, self.lo, self.hi, self.cur = nc, lo, hi, lo
        self.n = 0

    def alloc(self, name, shape, dtype):
        nbytes = int(np.prod(shape[1:])) * (4 if dtype == F32 else 2)
        nbytes = (nbytes + 63) // 64 * 64
        off = self.cur
        assert off + nbytes <= self.hi, (name, off, nbytes, self.hi)
        self.cur += nbytes
        self.n += 1
        return self.nc.alloc_sbuf_tensor_at(name, list(shape), dtype, offset=off)


def build_program(n_layers=2, dbg=None, phases=('A', 'B', 'C')):
    nc = bass.Bass("TRN2", target_bir_lowering=False)
    P = Prog(nc)
    dbg = dbg or ()

    def dt_in(name, shape, dtype=F32):
        return nc.dram_tensor(name, list(shape), dtype, kind="ExternalInput")

    def dt_scr(name, shape, dtype):
        kind = "ExternalOutput" if name in dbg else "Internal"
        return nc.dram_tensor(name, list(shape), dtype, kind=kind)

    x_in = dt_in("x", [S, D])
    ln_g = dt_in("ln_g", [2, 128, 8])
    w_in = dt_in("w_in", [2, D, DINX])
    b_f = dt_in("fox_b_f", [2, 8, 1])
    q_g = dt_in("mla_q_g", [2, 128, 2])
    w_uq = dt_in("mla_w_uq", [2, 256, 1024])
    kv_g = dt_in("mla_kv_g", [2, 128, 1])
    w_ukv = dt_in("mla_w_ukv", [2, 128, 1024])
    out_g = dt_in("out_g", [2, 128, 12])
    w_out = dt_in("w_out", [2, 1536, D])
    fin_g = dt_in("final_g", [D])
    c_ident = dt_in("c_ident", [128, 128])
    c_tri = dt_in("c_tri", [128, 128], BF16)
    c_cos = dt_in("c_cos", [128, S])
    c_sin = dt_in("c_sin", [128, S])
    c_ones = dt_in("c_ones", [8, 3, S], BF16)
    c_mq = dt_in("c_mq", [8, 6, S], BF16)
    c_mk = dt_in("c_mk", [8, 22, S], BF16)
    y_out = nc.dram_tensor("y", [S, D], F32, kind="ExternalOutput")

    fq_s = dt_scr("fq_s", [8, 70, S], BF16)
    fk_s = dt_scr("fk_s", [8, 70, S], BF16)
    fv_s = dt_scr("fv_s", [S, 8 * 65], BF16)
    bq_s = dt_scr("bq_s", [8, 86, S], BF16)
    bk_s = dt_scr("bk_s", [8, 86, S], BF16)
    bv_s = dt_scr("bv_s", [S, 8 * 65], BF16)
    mq1_s = dt_scr("mq1_s", [4, 128, S], BF16)
    mq2_s = dt_scr("mq2_s", [4, 64, S], BF16)
    mk1_s = dt_scr("mk1_s", [4, 128, S], BF16)
    mk2_s = dt_scr("mk2_s", [64, S], BF16)
    mv_s = dt_scr("mv_s", [S, 4 * 129], BF16)
    gz_s = dt_scr("gz_s", [S, 1536], F32)
    yy_s = dt_scr("yy_s", [S, 1536], BF16)
    xr_s = dt_scr("xr_s", [S, D], F32)

    LO, HI = 16512 + 64, 229344
    AP_ = Arena(nc, LO, HI)
    ident = AP_.alloc("ident", [128, 128], F32)
    identb = AP_.alloc("identb", [128, 128], BF16)
    tri = AP_.alloc("tri", [128, 128], BF16)
    onesf = AP_.alloc("onesf", [128, 128], F32)
    epsc = AP_.alloc("epsc", [128, 1], F32)
    kmT = AP_.alloc("kmT", [128, 4, 2, 16], F32)
    persist_end = AP_.cur
    WH = DINX // 2
    WA = Arena(nc, persist_end, HI)
    Wb = WA.alloc("Wb", [128, 8, DINX], BF16)
    wst = WA.alloc("wst", [128, WH], F32)
    gcol = WA.alloc("gcol", [128, 8], F32)
    wend = WA.cur
    TOPLO = HI - (12 * D * 2 + D * 4 + 64 + 64)
    TA = Arena(nc, TOPLO, HI)
    Wo = TA.alloc("Wo", [128, 12, D], BF16)
    wstC = TA.alloc("wstC", [128, D], F32)
    ogc = TA.alloc("ogc", [128, 12], F32)
    WB_K = lambda k: [('Wb', k, 0), ('Wb', k, 1)]

    def load_win(l_, pieces, stg):
        for n_, (k, hf) in enumerate(pieces):
            st_, rs_, sm_ = stg[n_ % len(stg)]
            P.dma('sp', sm_, lambda e: e.dma_start(
                out=st_[:, 0:WH], in_=w_in[l_, 128 * k:128 * (k + 1), WH * hf:WH * (hf + 1)]), w=[rs_])
            if hf:
                P.op('act', lambda e: e.activation(
                    out=Wb[:, k, WH * hf:WH * (hf + 1)], in_=st_[:, 0:WH], func=AF.Copy, scale=gcol[:, k:k + 1]),
                    r=[rs_, 'gcol'], w=[('Wb', k, hf)])
            else:
                P.op('dve', lambda e: e.tensor_scalar(
                    out=Wb[:, k, WH * hf:WH * (hf + 1)], in0=st_[:, 0:WH], scalar1=gcol[:, k:k + 1], scalar2=None,
                    op0=ALU.mult), r=[rs_, 'gcol'], w=[('Wb', k, hf)])

    def load_wout(l_, ks):
        for k in ks:
            if k == 0:
                P.dma('sp', 'wgo', lambda e: e.dma_start(out=ogc[:], in_=out_g[l_, :, :]), w=['ogc'])
            P.dma('sp', 'wstC', lambda e: e.dma_start(
                out=wstC[:], in_=w_out[l_, 128 * k:128 * (k + 1), :]), w=['wstC'])
            P.op('dve', lambda e: e.tensor_scalar(
                out=Wo[:, k, :], in0=wstC[:], scalar1=ogc[:, k:k + 1], scalar2=None, op0=ALU.mult),
                r=['wstC', 'ogc'], w=[('Wo', k)])

    ALLW = [(k, hf) for k in range(8) for hf in range(2)]
    ps = nc.alloc_psum_tensor("ps", [128, 4096], F32)
    psb = ps.bitcast(BF16)

    def bank(b, lo=0, hi=512):
        return ps[:, 512 * b + lo: 512 * b + hi]

    def PS(b):
        return ('ps', b)

    P.sec('init')
    P.dma('sp', 'c0', lambda e: e.dma_start(out=ident[:], in_=c_ident[:, :]), w=['ident'])
    P.dma('sp', 'c1', lambda e: e.dma_start(out=tri[:], in_=c_tri[:, :]), w=['tri'])
    P.op('dve', lambda e: e.tensor_copy(out=identb[:], in_=ident[:]), r=['ident'], w=['identb'])
    P.op('dve', lambda e: e.memset(onesf[:], 1.0), w=['onesf'])
    P.op('dve', lambda e: e.memset(epsc[:], EPS), w=['epsc'])
    P.op('dve', lambda e: e.memset(kmT[:], 0.0), w=[('kmT', j) for j in range(4)])
    P.dma('sp', 'c2', lambda e: e.dma_start(out=fq_s[:, 67:70, :], in_=c_ones[:, :, :]), w=[('fq', 'const')])
    P.dma('sp', 'c3', lambda e: e.dma_start(out=fk_s[:, 64:67, :], in_=c_ones[:, :, :]), w=[('fk', 'const')])
    P.dma('sp', 'c4', lambda e: e.dma_start(out=bq_s[:, 80:86, :], in_=c_mq[:, :, :]), w=[('bq', 'const')])
    P.dma('sp', 'c5', lambda e: e.dma_start(out=bk_s[:, 64:86, :], in_=c_mk[:, :, :]), w=[('bk', 'const')])

    for l in range(n_layers):
        x_src = x_in if l == 0 else xr_s
        last = (l == n_layers - 1)
        if 'A' in phases:
            P.barrier()
            A = Arena(nc, wend, HI)
            Wuq = A.alloc("Wuq", [128, 2, 1024], BF16)
            Wukv = A.alloc("Wukv", [128, 1024], BF16)
            gq = A.alloc("gq", [128, 2], F32)
            gkv = A.alloc("gkv", [128, 1], F32)
            bfc = A.alloc("bfc", [8, 1], F32)
            xin_off = A.cur
            xin = [A.alloc("xin0", [128, 4, D], F32)] * 2
            xT = A.alloc("xT", [128, 8, 512], BF16)
            sq = A.alloc("sq", [128, D], F32)
            ssqs = [A.alloc(f"ssq{i}", [128, 4], F32) for i in range(2)]
            rstds = [A.alloc(f"rstd{i}", [128, 4], F32) for i in range(2)]
            Rt = A.alloc("Rt", [128, 4, 128], F32)
            rbc = A.alloc("rbc", [128, 512], F32)
            fmst = [A.alloc(f"fmst{i}", [128, 512], BF16) for i in range(4)]
            qf = A.alloc("qf", [128, 4, 512], F32)
            kf = A.alloc("kf", [128, 512], F32)
            cqT = A.alloc("cqT", [128, 2, 512], F32)
            sq2 = A.alloc("sq2", [128, 2, 512], F32)
            rq = A.alloc("rq", [128, 512], F32)
            cqn = A.alloc("cqn", [128, 2, 512], BF16)
            ckvn = A.alloc("ckvn", [128, 512], BF16)
            cosc = A.alloc("cosc", [128, 512], F32)
            sinc = A.alloc("sinc", [128, 512], F32)
            t1 = A.alloc("t1", [128, 512], F32)
            t2 = A.alloc("t2", [128, 512], F32)
            ffx = A.alloc("ffx", [8, 512], F32)
            ffe = A.alloc("ffe", [8, 512], F32)
            ones8 = A.alloc("ones8", [8, 512], F32)
            cc = [A.alloc(f"cc{i}", [8, 512], F32) for i in range(2)]
            r1 = A.alloc("r1", [8, 512], F32)
            cs = A.alloc("cs", [8, 3, 512], BF16)
            ncs = A.alloc("ncs", [8, 3, 512], BF16)
            vst = A.alloc("vst", [128, 4, 8 * 65], BF16)
            vst2 = A.alloc("vst2", [128, 4, 8 * 65], BF16)
            mvst = A.alloc("mvst", [128, 4, 4 * 129], BF16)
            gzst = A.alloc("gzst", [128, 1536], F32)
            gate = A.alloc("gate", [128, 8, 16], F32)
            mx8 = A.alloc("mx8", [128, 8, 8], F32)
            masks = [A.alloc(f"mask{i}", [128, 8, 16], BF16) for i in range(4)]
            maskT = A.alloc("maskT", [16, 8, 128], BF16)

            P.sec('weights')
            if l == 0:
                wst2 = nc.alloc_sbuf_tensor_at("wst2", [128, WH], F32, offset=xin_off)
                P.dma('sp', 'wg', lambda e: e.dma_start(out=gcol[:], in_=ln_g[l, :, :]), w=['gcol'])
                load_win(0, ALLW, [(wst, 'wst', 'wst'), (wst2, ('xin', 0), 'xin0')])
            WB = [('Wb', k, j) for k in range(8) for j in range(2)]
            P.dma('sp', 'wg2', lambda e: e.dma_start(
                out=gq[:], in_=q_g[l, :, :]), w=['gq'])
            P.dma('sp', 'wg3', lambda e: e.dma_start(
                out=gkv[:], in_=kv_g[l, :, :]), w=['gkv'])
            P.dma('sp', 'wg4', lambda e: e.dma_start(
                out=bfc[:], in_=b_f[l, :, :]), w=['bfc'])
            for k in range(2):
                P.dma('sp', 'wst', lambda e, k=k: e.dma_start(
                    out=wst[:, 0:1024], in_=w_uq[l, 128 * k:128 * (k + 1), :]), w=['wst'])
                P.op('dve', lambda e, k=k: e.tensor_scalar(
                    out=Wuq[:, k, :], in0=wst[:, 0:1024], scalar1=gq[:, k:k + 1], scalar2=None,
                    op0=ALU.mult), r=['wst', 'gq'], w=['Wuq'])
            P.dma('sp', 'wst', lambda e: e.dma_start(
                out=wst[:, 0:1024], in_=w_ukv[l, :, :]), w=['wst'])
            P.op('dve', lambda e: e.tensor_scalar(
                out=Wukv[:], in0=wst[:, 0:1024], scalar1=gkv[:, 0:1], scalar2=None,
                op0=ALU.mult), r=['wst', 'gkv'], w=['Wukv'])
            P.op('dve', lambda e: e.memset(ones8[:], 1.0), w=['ones8'])
            P.op('dve', lambda e: e.memset(cc[1][:], 0.0), w=[('cc', 1)])
            P.op('pool', lambda e: e.memset(vst[:], 1.0), w=['vst'])
            P.op('pool', lambda e: e.memset(vst2[:], 1.0), w=['vst2'])
            P.op('pool', lambda e: e.memset(mvst[:], 1.0), w=['mvst'])

            pb = [0]

            def nb():
                pb[0] = (pb[0] + 1) % 8
                return pb[0]

            def store_pair(st, si, dst, j, tok):
                for hh in range(2):
                    P.dma('sp', f'fm{si}_{hh}', lambda e, hh=hh: e.dma_start(
                        out=dst[2 * j + hh, 0:64, tok], in_=st[64 * hh:64 * hh + 64, :]),
                        r=[('fmst', si)])

            def fm_job(col, ncols, rows_lo=0):
                b = nb()
                for k in range(8):
                    P.op('pe', lambda e, k=k, b=b: e.matmul(
                        bank(b)[rows_lo:rows_lo + ncols, :], lhsT=Wb[:, k, col:col + ncols],
                        rhs=xT[:, k, :], start=(k == 0), stop=(k == 7)),
                        r=WB_K(k) + ['xT'], w=[PS(b)])
                return b

            XI = ('xin', 0)
            xi = xin[0]
            qs = 192.0 ** -0.5

            def tokc(c):
                return slice(512 * c, 512 * (c + 1))

            def load_x(c):
                P.sec('pre')
                P.dma('sp', 'xin0', lambda e: e.dma_start(
                    out=xi[:], in_=x_src[tokc(c), :].rearrange("(t p) d -> p t d", p=128)), w=[XI])

            def load_trig(c):
                P.sec('mla')
                P.dma('sp', 'cosd', lambda e: e.dma_start(out=cosc[:], in_=c_cos[:, tokc(c)]), w=['cosc'])
                P.dma('sp', 'sind', lambda e: e.dma_start(out=sinc[:], in_=c_sin[:, tokc(c)]), w=['sinc'])

            def pre_stats(c):
                P.sec('pre')
                ssq, rstd = ssqs[c % 2], rstds[c % 2]
                SSQ, RSTD = ('ssq', c % 2), ('rstd', c % 2)
                for t in range(4):
                    P.op('dve', lambda e: e.tensor_tensor(
                        out=sq[:], in0=xi[:, t, :], in1=xi[:, t, :], op=ALU.mult), r=[XI], w=['sq'])
                    P.op('dve', lambda e: e.reduce_sum(
                        out=ssq[:, t:t + 1], in_=sq[:], axis=AX.X), r=['sq'], w=[SSQ])
                P.op('act', lambda e: e.activation(
                    out=ssq[:], in_=ssq[:], func=AF.Sqrt, bias=epsc[:, 0:1], scale=1.0 / D),
                    r=[SSQ, 'epsc'], w=[SSQ])
                P.op('dve', lambda e: e.reciprocal(out=rstd[:], in_=ssq[:]), r=[SSQ], w=[RSTD])
                for t in range(4):
                    P.op('dve', lambda e: e.tensor_scalar(
                        out=Rt[:, t, :], in0=onesf[:], scalar1=rstd[:, t:t + 1], scalar2=None, op0=ALU.mult),
                        r=['onesf', RSTD], w=[('Rt', t)], ss=True)

            def pre_pe(c):
                P.sec('pre')
                for k in range(8):
                    b = nb()
                    for t in range(4):
                        P.op('pe', lambda e: e.transpose(
                            out=bank(b, 128 * t, 128 * (t + 1)), in_=xi[:, t, 128 * k:128 * (k + 1)],
                            identity=ident[:]), r=[XI, 'ident'], w=[PS(b)])
                    if k % 2 == 0:
                        P.op('act', lambda e: e.copy(out=xT[:, k, :], in_=bank(b)), r=[PS(b)], w=['xT'])
                    else:
                        P.op('dve', lambda e: e.tensor_copy(out=xT[:, k, :], in_=bank(b)), r=[PS(b)], w=['xT'])
                b = nb()
                for t in range(4):
                    P.op('pe', lambda e: e.matmul(
                        bank(b, 128 * t, 128 * (t + 1)), lhsT=Rt[:, t, :], rhs=ident[:], start=True, stop=True),
                        r=[('Rt', t), 'ident'], w=[PS(b)])
                P.op('act', lambda e: e.copy(out=rbc[:], in_=bank(b)), r=[PS(b)], w=['rbc'])

            def s_fox(c):
                P.sec('fox')
                tok = tokc(c)
                for j in range(4):
                    b = fm_job(C_FQ + 128 * j, 128)
                    st = fmst[j % 4]
                    P.op('dve', lambda e: e.scalar_tensor_tensor(
                        out=st[:], in0=bank(b), scalar=0.125, in1=rbc[:], op0=ALU.mult, op1=ALU.mult),
                        r=[PS(b), 'rbc'], w=[('fmst', j % 4)])
                    store_pair(st, j % 4, fq_s, j, tok)
                for j in range(4):
                    b = fm_job(C_FK + 128 * j, 128)
                    st = fmst[j % 4]
                    P.op('dve', lambda e: e.tensor_tensor(
                        out=st[:], in0=bank(b), in1=rbc[:], op=ALU.mult),
                        r=[PS(b), 'rbc'], w=[('fmst', j % 4)])
                    store_pair(st, j % 4, fk_s, j, tok)

            def s_ff(c):
                P.sec('ff')
                tok = tokc(c)
                b = fm_job(C_FF, 8)
                P.op('dve', lambda e: e.tensor_tensor(
                    out=ffx[:], in0=bank(b)[0:8, :], in1=rbc[0:8, :], op=ALU.mult),
                    r=[PS(b), 'rbc'], w=['ffx'])
                P.op('dve', lambda e: e.tensor_scalar(
                    out=ffx[:], in0=ffx[:], scalar1=bfc[:, 0:1], scalar2=-1.0, op0=ALU.add, op1=ALU.mult),
                    r=['ffx', 'bfc'], w=['ffx'])
                P.op('act', lambda e: e.activation(out=ffe[:], in_=ffx[:], func=AF.Exp), r=['ffx'], w=['ffe'])
                P.op('act', lambda e: e.activation(out=ffe[:], in_=ffe[:], func=AF.Ln, bias=onesf[0:8, 0:1]),
                     r=['ffe', 'onesf'], w=['ffe'], ss=True)

            def s_ff_b(c):
                P.sec('ff')
                tok = tokc(c)
                cprev, ccur = cc[(c + 1) % 2], cc[c % 2]
                if c == 0:
                    P.op('dve', lambda e: e.tensor_tensor_scan(
                        out=ccur[:], data0=ones8[:], data1=ffe[:], initial=0.0,
                        op0=ALU.mult, op1=ALU.subtract), r=['ones8', 'ffe'], w=[('cc', c % 2)])
                else:
                    P.op('dve', lambda e: e.tensor_tensor_scan(
                        out=ccur[:], data0=ones8[:], data1=ffe[:], initial=cprev[:, 511:512],
                        op0=ALU.mult, op1=ALU.subtract),
                        r=['ones8', 'ffe', ('cc', (c + 1) % 2)], w=[('cc', c % 2)], ss=True)
                P.op('dve', lambda e: e.tensor_copy(out=cs[:, 0, :], in_=ccur[:]), r=[('cc', c % 2)], w=['cs'])
                P.op('dve', lambda e: e.tensor_tensor(
                    out=r1[:], in0=ccur[:], in1=cs[:, 0, :], op=ALU.subtract),
                    r=[('cc', c % 2), 'cs'], w=['r1'])
                P.op('dve', lambda e: e.tensor_copy(out=cs[:, 1, :], in_=r1[:]), r=['r1'], w=['cs'])
                P.op('dve', lambda e: e.tensor_tensor(
                    out=r1[:], in0=r1[:], in1=cs[:, 1, :], op=ALU.subtract), r=['r1', 'cs'], w=['r1'])
                P.op('dve', lambda e: e.tensor_copy(out=cs[:, 2, :], in_=r1[:]), r=['r1'], w=['cs'])
                P.op('dve', lambda e: e.tensor_scalar(
                    out=ncs[:], in0=cs[:], scalar1=-1.0, scalar2=None, op0=ALU.mult), r=['cs'], w=['ncs'])
                P.dma('sp', 'csd', lambda e: e.dma_start(out=fq_s[:, 64:67, tok], in_=cs[:]), r=['cs'])
                P.dma('sp', 'ncsd', lambda e: e.dma_start(out=fk_s[:, 67:70, tok], in_=ncs[:]), r=['ncs'])

            def s_moba(c):
                P.sec('moba')
                tok = tokc(c)
                for j in range(4):
                    b = fm_job(C_BQ + 128 * j, 128)
                    st = fmst[j % 4]
                    P.op('dve', lambda e: e.scalar_tensor_tensor(
                        out=qf[:, j, :], in0=bank(b), scalar=0.125, in1=rbc[:], op0=ALU.mult, op1=ALU.mult),
                        r=[PS(b), 'rbc'], w=[('qf', j)])
                    P.op('pool', lambda e: e.tensor_copy(out=st[:], in_=qf[:, j, :]),
                         r=[('qf', j)], w=[('fmst', j % 4)])
                    store_pair(st, j % 4, bq_s, j, tok)
                for j in range(4):
                    b = fm_job(C_BK + 128 * j, 128)
                    st = fmst[j % 4]
                    P.op('dve', lambda e: e.tensor_tensor(
                        out=kf[:], in0=bank(b), in1=rbc[:], op=ALU.mult), r=[PS(b), 'rbc'], w=['kf'])
                    for hh in range(2):
                        P.op('dve', lambda e: e.reduce_sum(
                            out=kmT[64 * hh:64 * hh + 64, j, hh, 2 * c:2 * c + 2],
                            in_=kf[64 * hh:64 * hh + 64, :].rearrange("p (n s) -> p n s", s=256),
                            axis=AX.X), r=['kf'], w=[('kmT', j)])
                    P.op('pool', lambda e: e.tensor_copy(out=st[:], in_=kf[:]),
                         r=['kf'], w=[('fmst', j % 4)])
                    store_pair(st, j % 4, bk_s, j, tok)

            gate_bank = [0]

            def s_gate_mm(c):
                P.sec('gate')
                b = nb()
                gate_bank[0] = b
                for t in range(4):
                    for j in range(4):
                        P.op('pe', lambda e: e.matmul(
                            bank(b, 128 * t + 32 * j, 128 * t + 32 * j + 32),
                            lhsT=qf[:, j, 128 * t:128 * (t + 1)],
                            rhs=kmT[:, j, :, :].rearrange("p a n -> p (a n)"), start=True, stop=True),
                            r=[('qf', j), ('kmT', j)], w=[PS(b)])

            def s_gate_dve(c):
                P.sec('gate_top')
                b = gate_bank[0]
                for t in range(4):
                    cur = 2 * c + t // 2
                    mk = masks[t]
                    MK = ('mask', t)
                    P.op('dve', lambda e: e.memset(gate[:], NEG), w=['gate'])
                    if cur > 0:
                        P.op('dve', lambda e: e.tensor_copy(
                            out=gate[:, :, 0:cur],
                            in_=bank(b, 128 * t, 128 * (t + 1)).rearrange("p (h n) -> p h n", n=16)[:, :, 0:cur]),
                            r=[PS(b)], w=['gate'])
                    for h in range(8):
                        P.op('dve', lambda e: e.max(out=mx8[:, h, :], in_=gate[:, h, :]), r=['gate'], w=['mx8'])
                    for h in range(8):
                        P.op('dve', lambda e: e.tensor_scalar(
                            out=mk[:, h, :], in0=gate[:, h, :], scalar1=mx8[:, h, 2:3], scalar2=NEG,
                            op0=ALU.is_lt, op1=ALU.mult), r=['gate', 'mx8'], w=[MK], ss=True)
                    if cur <= 3 and cur > 0:
                        P.op('dve', lambda e: e.memset(mk[:, :, 0:cur], 0.0), w=[MK])
                    P.op('dve', lambda e: e.memset(mk[:, :, cur:cur + 1], 0.0), w=[MK])
                    if cur < 15:
                        P.op('dve', lambda e: e.memset(mk[:, :, cur + 1:16], NEG), w=[MK])

            def s_gate_tr(c):
                P.sec('gate_tr')
                for t in range(4):
                    mk = masks[t]
                    b2 = nb()
                    for h in range(8):
                        P.op('pe', lambda e: e.transpose(
                            out=psb[0:16, 1024 * b2 + 128 * h: 1024 * b2 + 128 * (h + 1)],
                            in_=mk[:, h, :], identity=identb[:]),
                            r=[('mask', t), 'identb'], w=[PS(b2)])
                    P.op('act', lambda e: e.copy(
                        out=maskT[:],
                        in_=psb[0:16, 1024 * b2:1024 * b2 + 1024].rearrange("p (h q) -> p h q", q=128)),
                        r=[PS(b2)], w=['maskT'])
                    P.dma('sp', 'mskd', lambda e: e.dma_start(
                        out=bq_s[:, 64:80, 512 * c + 128 * t:512 * c + 128 * (t + 1)].rearrange("h n s -> n h s"),
                        in_=maskT[:]), r=['maskT'])

            def s_mla_q_fm(c):
                P.sec('mla')
                for j in range(2):
                    b = fm_job(C_CQ + 128 * j, 128)
                    P.op('dve', lambda e: e.tensor_tensor(
                        out=cqT[:, j, :], in0=bank(b), in1=rbc[:], op=ALU.mult),
                        r=[PS(b), 'rbc'], w=[('cqT', j)])
                    P.op('act', lambda e: e.activation(out=sq2[:, j, :], in_=cqT[:, j, :], func=AF.Square),
                         r=[('cqT', j)], w=[('sq2', j)])

            def s_mla_q_norm(c):
                P.sec('mla')
                b = nb()
                for j in range(2):
                    P.op('pe', lambda e: e.matmul(
                        bank(b), lhsT=onesf[:], rhs=sq2[:, j, :], start=(j == 0), stop=(j == 1)),
                        r=['onesf', ('sq2', j)], w=[PS(b)])
                P.op('act', lambda e: e.activation(
                    out=rq[:], in_=bank(b), func=AF.Sqrt, bias=epsc[:, 0:1], scale=1.0 / 256),
                    r=[PS(b), 'epsc'], w=['rq'])
                P.op('dve', lambda e: e.reciprocal(out=rq[:], in_=rq[:]), r=['rq'], w=['rq'])
                for j in range(2):
                    P.op('dve', lambda e: e.tensor_tensor(
                        out=cqn[:, j, :], in0=cqT[:, j, :], in1=rq[:], op=ALU.mult),
                        r=[('cqT', j), 'rq'], w=['cqn'])

            def s_mla_kv_fm(c):
                P.sec('mla')
                tok = tokc(c)
                b = fm_job(C_CKV, 128)
                P.op('dve', lambda e: e.tensor_tensor(
                    out=cqT[:, 0, :], in0=bank(b), in1=rbc[:], op=ALU.mult), r=[PS(b), 'rbc'], w=[('cqT', 0)])
                P.op('act', lambda e: e.activation(out=sq2[:, 0, :], in_=cqT[:, 0, :], func=AF.Square),
                     r=[('cqT', 0)], w=[('sq2', 0)])
                bA = fm_job(C_KR, 64)
                bB = fm_job(C_KRS, 64)
                P.op('dve', lambda e: e.tensor_tensor(
                    out=t1[0:64, :], in0=bank(bA)[0:64, :], in1=rbc[0:64, :], op=ALU.mult),
                    r=[PS(bA), 'rbc'], w=['t1'])
                P.op('dve', lambda e: e.tensor_tensor(
                    out=t2[0:64, :], in0=bank(bB)[0:64, :], in1=rbc[0:64, :], op=ALU.mult),
                    r=[PS(bB), 'rbc'], w=['t2'])
                P.op('pool', lambda e: e.tensor_tensor(
                    out=t1[0:64, :], in0=t1[0:64, :], in1=cosc[0:64, :], op=ALU.mult),
                    r=['t1', 'cosc'], w=['t1'])
                P.op('pool', lambda e: e.tensor_tensor(
                    out=t2[0:64, :], in0=t2[0:64, :], in1=sinc[0:64, :], op=ALU.mult),
                    r=['t2', 'sinc'], w=['t2'])
                st = fmst[2]
                P.op('pool', lambda e: e.tensor_tensor(
                    out=st[0:64, :], in0=t1[0:64, :], in1=t2[0:64, :], op=ALU.add),
                    r=['t1', 't2'], w=[('fmst', 2)])
                P.dma('sp', 'fm2', lambda e: e.dma_start(
                    out=mk2_s[:, tok], in_=st[0:64, :]), r=[('fmst', 2)])

            def s_mla_kv_norm(c):
                P.sec('mla')
                b = nb()
                P.op('pe', lambda e: e.matmul(
                    bank(b), lhsT=onesf[:], rhs=sq2[:, 0, :], start=True, stop=True),
                    r=['onesf', ('sq2', 0)], w=[PS(b)])
                P.op('act', lambda e: e.activation(
                    out=rq[:], in_=bank(b), func=AF.Sqrt, bias=epsc[:, 0:1], scale=1.0 / 128),
                    r=[PS(b), 'epsc'], w=['rq'])
                P.op('dve', lambda e: e.reciprocal(out=rq[:], in_=rq[:]), r=['rq'], w=['rq'])
                P.op('dve', lambda e: e.tensor_tensor(
                    out=ckvn[:], in0=cqT[:, 0, :], in1=rq[:], op=ALU.mult), r=[('cqT', 0), 'rq'], w=['ckvn'])

            def s_mla_q_up(c):
                P.sec('mla')
                tok = tokc(c)
                for h in range(4):
                    b = nb()
                    for k in range(2):
                        P.op('pe', lambda e: e.matmul(
                            bank(b), lhsT=Wuq[:, k, 128 * h:128 * (h + 1)], rhs=cqn[:, k, :],
                            start=(k == 0), stop=(k == 1)), r=['Wuq', 'cqn'], w=[PS(b)])
                    st = fmst[h % 4]
                    P.op('act', lambda e: e.mul(out=st[:], in_=bank(b), mul=qs),
                         r=[PS(b)], w=[('fmst', h % 4)])
                    P.dma('sp', f'fm{h % 4}', lambda e: e.dma_start(
                        out=mq1_s[h, :, tok], in_=st[:]), r=[('fmst', h % 4)])
                for p in range(2):
                    bA, bB = nb(), nb()
                    for k in range(2):
                        P.op('pe', lambda e: e.matmul(
                            bank(bA), lhsT=Wuq[:, k, 512 + 128 * p:512 + 128 * (p + 1)], rhs=cqn[:, k, :],
                            start=(k == 0), stop=(k == 1)), r=['Wuq', 'cqn'], w=[PS(bA)])
                    for k in range(2):
                        P.op('pe', lambda e: e.matmul(
                            bank(bB), lhsT=Wuq[:, k, 768 + 128 * p:768 + 128 * (p + 1)], rhs=cqn[:, k, :],
                            start=(k == 0), stop=(k == 1)), r=['Wuq', 'cqn'], w=[PS(bB)])
                    P.op('dve', lambda e: e.tensor_tensor(
                        out=t1[:], in0=bank(bA), in1=cosc[:], op=ALU.mult), r=[PS(bA), 'cosc'], w=['t1'])
                    P.op('dve', lambda e: e.tensor_tensor(
                        out=t2[:], in0=bank(bB), in1=sinc[:], op=ALU.mult), r=[PS(bB), 'sinc'], w=['t2'])
                    P.op('pool', lambda e: e.tensor_tensor(
                        out=t1[:], in0=t1[:], in1=t2[:], op=ALU.add), r=['t1', 't2'], w=['t1'])
                    st = fmst[p]
                    P.op('act', lambda e: e.mul(out=st[:], in_=t1[:], mul=qs), r=['t1'], w=[('fmst', p)])
                    store_pair(st, p, mq2_s, p, tok)

            def s_mla_kv_up(c):
                P.sec('mla')
                tok = tokc(c)
                for h in range(4):
                    b = nb()
                    P.op('pe', lambda e: e.matmul(
                        bank(b), lhsT=Wukv[:, 128 * h:128 * (h + 1)], rhs=ckvn[:], start=True, stop=True),
                        r=['Wukv', 'ckvn'], w=[PS(b)])
                    st = fmst[h % 4]
                    P.op('act', lambda e: e.copy(out=st[:], in_=bank(b)), r=[PS(b)], w=[('fmst', h % 4)])
                    P.dma('sp', f'fm{h % 4}', lambda e: e.dma_start(
                        out=mk1_s[h, :, tok], in_=st[:]), r=[('fmst', h % 4)])
                for t in range(4):
                    b = nb()
                    P.op('pe', lambda e: e.matmul(
                        bank(b), lhsT=ckvn[:, 128 * t:128 * (t + 1)], rhs=Wukv[:, 512:1024],
                        start=True, stop=True), r=['Wukv', 'ckvn'], w=[PS(b)])
                    P.op('act', lambda e: e.copy(
                        out=mvst[:, t, :].rearrange("p (h d) -> p h d", d=129)[:, :, 0:128],
                        in_=bank(b).rearrange("p (h d) -> p h d", d=128)), r=[PS(b)], w=['mvst'])
                P.dma('sp', 'mvd', lambda e: e.dma_start(
                    out=mv_s[tok, :].rearrange("(t p) f -> p t f", p=128), in_=mvst[:]), r=['mvst'])

            def s_tm(c, tiles):
                P.sec('tm')
                for t in tiles:
                    for (col, kind) in ((C_FV, 'fv'), (C_BV, 'bv'), (C_FZ, 'z0'), (C_MZ, 'z1'), (C_BZ, 'z2')):
                        b = nb()
                        for k in range(8):
                            P.op('pe', lambda e: e.matmul(
                                bank(b), lhsT=xT[:, k, 128 * t:128 * (t + 1)], rhs=Wb[:, k, col:col + 512],
                                start=(k == 0), stop=(k == 7)), r=WB_K(k) + ['xT'], w=[PS(b)])
                        if kind in ('fv', 'bv'):
                            vs = vst if kind == 'fv' else vst2
                            P.op('act', lambda e: e.activation(
                                out=vs[:, t, :].rearrange("p (h d) -> p h d", d=65)[:, :, 0:64],
                                in_=bank(b).rearrange("p (h d) -> p h d", d=64), func=AF.Copy,
                                scale=rstds[c % 2][:, t:t + 1]), r=[PS(b), ('rstd', c % 2)],
                                w=['vst' if kind == 'fv' else 'vst2'])
                        else:
                            g = int(kind[1])
                            P.op('act', lambda e: e.activation(
                                out=gzst[:, 512 * g:512 * (g + 1)], in_=bank(b), func=AF.Silu,
                                scale=rstds[c % 2][:, t:t + 1]), r=[PS(b), ('rstd', c % 2)], w=['gzst'])
                    P.dma('sp', 'gzd', lambda e: e.dma_start(
                        out=gz_s[512 * c + 128 * t:512 * c + 128 * (t + 1), :], in_=gzst[:]), r=['gzst'])

            def s_vstore(c):
                P.sec('tm')
                tok = tokc(c)
                P.dma('sp', 'fvd', lambda e: e.dma_start(
                    out=fv_s[tok, :].rearrange("(t p) f -> p t f", p=128), in_=vst[:]), r=['vst'])
                P.dma('sp', 'bvd', lambda e: e.dma_start(
                    out=bv_s[tok, :].rearrange("(t p) f -> p t f", p=128), in_=vst2[:]), r=['vst2'])

            NCR = NCH_RUN[0]
            load_x(0)
            pre_stats(0)
            pre_pe(0)
            for c in range(NCR):
                if c + 1 < NCR:
                    load_x(c + 1)
                load_trig(c)
                s_fox(c)
                s_ff(c)
                s_moba(c)
                s_ff_b(c)
                s_mla_q_fm(c)
                s_tm(c, (0,))
                s_gate_mm(c)
                s_gate_dve(c)
                s_mla_q_norm(c)
                if c + 1 < NCR:
                    pre_stats(c + 1)
                s_tm(c, (1,))
                s_mla_kv_fm(c)
                s_tm(c, (2,))
                s_mla_kv_norm(c)
                s_tm(c, (3,))
                s_vstore(c)
                if c + 1 < NCR:
                    pre_pe(c + 1)
                s_mla_q_up(c)
                s_mla_kv_up(c)
                s_gate_tr(c)

        if 'B' in phases:
            mixers = (
                dict(name='fox', H=8, dv=64, rows=[70], q=[fq_s], k=[fk_s], v=fv_s, g=0,
                     qres=lambda c: [('fq', 'const'), ('fq', c, 'c')] + [('fq', c, j) for j in range(4)],
                     kres=lambda c: [('fk', 'const'), ('fk', c, 'c')] + [('fk', c, j) for j in range(4)],
                     vres=lambda c: [('fv', c)]),
                dict(name='mla', H=4, dv=128, rows=[128, 64], q=[mq1_s, mq2_s], k=[mk1_s, mk2_s], v=mv_s, g=1,
                     qres=lambda c: [('mq1', c, h) for h in range(4)] + [('mq2', c, p) for p in range(2)],
                     kres=lambda c: [('mk1', c, h) for h in range(4)] + [('mk2', c)],
                     vres=lambda c: [('mv', c)]),
                dict(name='moba', H=8, dv=64, rows=[86], q=[bq_s], k=[bk_s], v=bv_s, g=2,
                     qres=lambda c: [('bq', 'const'), ('bq', c, 'm')] + [('bq', c, j) for j in range(4)],
                     kres=lambda c: [('bk', 'const')] + [('bk', c, j) for j in range(4)],
                     vres=lambda c: [('bv', c)]),
            )
            for mx in mixers:
                P.barrier()
                B = Arena(nc, persist_end, HI)
                H, dv, rows = mx['H'], mx['dv'], mx['rows']
                dv1 = dv + 1
                np_ = len(rows)
                nm = mx['name']
                G = mx['g']
                GB = 3 if 4 * dv1 <= 512 else 2
                kT = [B.alloc(f"kT{i}", [rows[i], H if not (nm == 'mla' and i == 1) else 1, S], BF16)
                      for i in range(np_)]
                vv = B.alloc("vv", [128, NT, H * dv1], BF16)
                qT = [[B.alloc(f"qT{i}_{bf}", [rows[i], H, 512], BF16) for i in range(np_)] for bf in range(2)]
                pT = [B.alloc(f"pT{i}", [128, GB * 512], BF16) for i in range(3)]
                oo = B.alloc("oo", [128, 4, 512], F32)
                gzt = [B.alloc(f"gzt{i}", [128, 4, 512], F32) for i in range(3)]
                rcp = B.alloc("rcp", [128, 4], F32)
                osq = B.alloc("osq", [128, 512], F32)
                oss = B.alloc("oss", [128, 4], F32)
                ors = B.alloc("ors", [128, 4], F32)
                yst = B.alloc("yst", [128, 4, 512], BF16)

                if GB == 3:
                    def acc(a_, u):
                        base = 512 * (6 + a_)
                        return ps[:, base + dv1 * u: base + dv1 * (u + 1)]

                    def ACC(a_):
                        return [('ps', 6 + a_)]

                    def acc_all(a_):
                        return ps[:, 512 * (6 + a_): 512 * (6 + a_) + 4 * dv1]
                else:
                    def acc(a_, u):
                        base = 2048 + 1024 * a_ + 512 * (u // 2) + dv1 * (u % 2)
                        return ps[:, base: base + dv1]

                    def ACC(a_):
                        return [('ps', 4 + 2 * a_), ('ps', 5 + 2 * a_)]

                    def acc_all(a_):
                        return ps[:, 2048 + 1024 * a_: 2048 + 1024 * a_ + 512 + 2 * dv1]

                def loads(c):
                    tok = slice(512 * c, 512 * (c + 1))
                    qb = qT[c % 2]
                    for i in range(np_):
                        if nm == 'mla' and i == 1:
                            P.dma('sp', f'k{i}', lambda e: e.dma_start(
                                out=kT[i][:, 0, tok], in_=mx['k'][i][:, tok]), w=[(nm + 'kT', i, c)])
                        else:
                            P.dma('sp', f'k{i}', lambda e: e.dma_start(
                                out=kT[i][:, :, tok], in_=mx['k'][i][:, :, tok].rearrange("h r s -> r h s")),
                                w=[(nm + 'kT', i, c)])
                        P.dma('sp', f'q{i}{c % 2}', lambda e: e.dma_start(
                            out=qb[i][:], in_=mx['q'][i][:, :, tok].rearrange("h r s -> r h s")),
                            w=[(nm + 'qT', c % 2, i)])
                    P.dma('sp', 'vl', lambda e: e.dma_start(
                        out=vv[:, 4 * c:4 * c + 4, :], in_=mx['v'][tok, :].rearrange("(t p) f -> p t f", p=128)),
                        w=[(nm + 'vv', c)])
                    P.dma('sp', f'gl{c % 3}', lambda e: e.dma_start(
                        out=gzt[c % 3][:], in_=gz_s[tok, 512 * G:512 * (G + 1)].rearrange("(t p) f -> p t f", p=128)),
                        w=[('gzt', c % 3)])

                groups = []
                hc = 0
                for c in range(NCH):
                    for h in range(H):
                        full = [(kt, 0, 0) for kt in range(4 * c)]
                        gl = [[(kt, 0, 512 * i) for i, (kt, _, _) in enumerate(full[i0:i0 + GB])]
                              for i0 in range(0, len(full), GB)]
                        if GB == 3:
                            gl.append([(4 * c + 0, 0, 0), (4 * c + 1, 1, 512), (4 * c + 3, 3, 896), (4 * c + 2, 2, 1024)])
                        else:
                            gl.append([(4 * c + 0, 0, 0), (4 * c + 1, 1, 512)])
                            gl.append([(4 * c + 2, 2, 0), (4 * c + 3, 3, 256)])
                        for gi, g in enumerate(gl):
                            groups.append(dict(c=c, h=h, tiles=g, fh=(gi == 0), lh=(gi == len(gl) - 1), aset=hc % 2))
                        hc += 1

                def emit_score(g, slot):
                    c, h = g['c'], g['h']
                    qb = qT[c % 2]
                    if g['fh']:
                        P.op('dve', lambda e: e.memset(acc_all(g['aset']), 0.0), w=ACC(g['aset']))
                    for ti, (kt, u0, off) in enumerate(g['tiles']):
                        b = slot * GB + off // 512
                        lo = 512 * slot * GB + off
                        wd = 512 - 128 * u0
                        diag = kt >= 4 * c
                        for i in range(np_):
                            hk = 0 if (nm == 'mla' and i == 1) else h
                            P.op('pe', lambda e: e.matmul(
                                ps[:, lo:lo + wd], lhsT=kT[i][:, hk, 128 * kt:128 * (kt + 1)],
                                rhs=qb[i][:, h, 128 * u0:512], start=(i == 0),
                                stop=(i == np_ - 1 and not diag), skip_group_check=diag),
                                r=[(nm + 'kT', i, kt // 4), (nm + 'qT', c % 2, i)], w=[PS(b)])
                        if diag:
                            P.op('pe', lambda e: e.matmul(
                                ps[:, lo:lo + 128], lhsT=identb[:], rhs=tri[:],
                                start=False, stop=True, skip_group_check=True),
                                r=['identb', 'tri'], w=[PS(b)])

                def emit_exp(g, slot, pb_):
                    tl = g['tiles']
                    wtot = max(off + 512 - 128 * u0 for (_, u0, off) in tl)
                    lo = 512 * slot * GB
                    banks = sorted(set(slot * GB + off // 512 for (_, _, off) in tl))
                    P.op('act', lambda e: e.activation(
                        out=pT[pb_][:, 0:wtot], in_=ps[:, lo:lo + wtot], func=AF.Exp),
                        r=[PS(b_) for b_ in banks], w=[('pT', pb_)])

                def emit_pv(g, pb_):
                    h, a_ = g['h'], g['aset']
                    for ti, (kt, u0, off) in enumerate(g['tiles']):
                        for u in range(u0, 4):
                            P.op('pe', lambda e: e.matmul(
                                acc(a_, u), lhsT=pT[pb_][:, off + 128 * (u - u0):off + 128 * (u - u0 + 1)],
                                rhs=vv[:, kt, dv1 * h:dv1 * (h + 1)], start=False, stop=False,
                                skip_group_check=True),
                                r=[('pT', pb_), (nm + 'vv', kt // 4)], w=ACC(a_))

                def head_epilogue(g):
                    h, a_ = g['h'], g['aset']
                    for u in range(4):
                        P.op('dve', lambda e: e.reciprocal(
                            out=rcp[:, u:u + 1], in_=acc(a_, u)[:, dv:dv1]), r=ACC(a_), w=['rcp'])
                    for u in range(4):
                        P.op('dve', lambda e: e.tensor_scalar(
                            out=oo[:, u, dv * h:dv * (h + 1)], in0=acc(a_, u)[:, 0:dv],
                            scalar1=rcp[:, u:u + 1], scalar2=None, op0=ALU.mult),
                            r=ACC(a_) + ['rcp'], w=['oo'], ss=True)

                def chunk_epilogue_a(c):
                    for u in range(4):
                        P.op('dve', lambda e: e.tensor_tensor(
                            out=osq[:], in0=oo[:, u, :], in1=oo[:, u, :], op=ALU.mult), r=['oo'], w=['osq'])
                        P.op('dve', lambda e: e.reduce_sum(out=oss[:, u:u + 1], in_=osq[:], axis=AX.X),
                             r=['osq'], w=['oss'])

                def chunk_epilogue_b(c):
                    tok = slice(512 * c, 512 * (c + 1))
                    P.op('act', lambda e: e.activation(
                        out=oss[:], in_=oss[:], func=AF.Ln, bias=epsc[:, 0:1], scale=1.0 / 512),
                        r=['oss', 'epsc'], w=['oss'])
                    P.op('act', lambda e: e.activation(out=ors[:], in_=oss[:], func=AF.Exp, scale=-0.5),
                         r=['oss'], w=['ors'], ss=True)
                    for u in range(4):
                        P.op('dve', lambda e: e.scalar_tensor_tensor(
                            out=yst[:, u, :], in0=oo[:, u, :], scalar=ors[:, u:u + 1], in1=gzt[c % 3][:, u, :],
                            op0=ALU.mult, op1=ALU.mult), r=['oo', 'ors', ('gzt', c % 3)], w=['yst'], ss=True)
                    P.dma('pool', 'yd', lambda e: e.dma_start(
                        out=yy_s[tok, 512 * G:512 * (G + 1)].rearrange("(t p) f -> p t f", p=128), in_=yst[:]),
                        r=['yst'])

                loads(0)
                emit_score(groups[0], 0)
                ng = len(groups)
                pend = []
                for gi, g in enumerate(groups):
                    if pend and pend[0][0] <= gi:
                        chunk_epilogue_b(pend.pop(0)[1])
                    if g['fh'] and g['h'] == 0 and g['c'] + 1 < NCH:
                        loads(g['c'] + 1)
                    if g['fh'] and g['h'] == 0 and nm == 'moba' and 'C' in phases and g['c'] < 6:
                        load_wout(l, range(2 * g['c'], 2 * g['c'] + 2))
                    emit_exp(g, gi % 2, gi % 3)
                    if gi + 1 < ng:
                        emit_score(groups[gi + 1], (gi + 1) % 2)
                    emit_pv(g, gi % 3)
                    if g['lh']:
                        head_epilogue(g)
                        if g['h'] == H - 1:
                            chunk_epilogue_a(g['c'])
                            pend.append((gi + 3, g['c']))
                for _, c_ in pend:
                    chunk_epilogue_b(c_)

        if 'C' in phases:
            P.barrier()
            Cc = Arena(nc, wend, TOPLO)
            wstn = Cc.alloc("wstn", [128, WH], F32)
            yin = Cc.alloc("yin", [128, 4, 1536], BF16)
            yT = Cc.alloc("yT", [128, 12, 512], BF16)
            xin = Cc.alloc("xinC", [128, 4, D], F32)
            x1 = Cc.alloc("x1", [128, 4, D], F32)
            fg = Cc.alloc("fg", [128, D], F32)
            sq = Cc.alloc("sqC", [128, D], F32)
            ssq = Cc.alloc("ssqC", [128, 4], F32)
            rstd = Cc.alloc("rstdC", [128, 4], F32)
            if 'B' not in phases:
                load_wout(l, range(12))
            if not last:
                P.dma('sp', 'wg', lambda e: e.dma_start(out=gcol[:], in_=ln_g[l + 1, :, :]), w=['gcol'])
            WO = [('Wo', k) for k in range(12)]
            if last:
                P.dma('sp', 'wg2', lambda e: e.dma_start(
                    out=fg[:], in_=fin_g.ap().partition_broadcast(128)), w=['fg'])
            pb = [0]
            for c in range(NCH):
                tok = slice(512 * c, 512 * (c + 1))
                if not last:
                    load_win(l + 1, ALLW[2 * c:2 * c + 2], [(wst, 'wst', 'wst'), (wstn, 'wstn', 'wstn')])
                P.dma('sp', 'yl', lambda e, tok=tok: e.dma_start(
                    out=yin[:], in_=yy_s[tok, :].rearrange("(t p) f -> p t f", p=128)),
                    r=[('yy', c, g) for g in range(3)], w=['yin'])
                P.dma('sp', 'xl', lambda e, tok=tok: e.dma_start(
                    out=xin[:], in_=x_src[tok, :].rearrange("(t p) d -> p t d", p=128)), w=['xinC'])
                for k in range(12):
                    b = pb[0] % 4
                    pb[0] += 1
                    for t in range(4):
                        P.op('pe', lambda e, k=k, t=t, b=b: e.transpose(
                            out=psb[:, 1024 * b + 128 * t:1024 * b + 128 * (t + 1)],
                            in_=yin[:, t, 128 * k:128 * (k + 1)], identity=identb[:]),
                            r=['yin', 'identb'], w=[PS(b)])
                    P.op('act' if k % 2 else 'dve',
                         (lambda e, k=k, b=b: e.copy(out=yT[:, k, :], in_=psb[:, 1024 * b:1024 * b + 512])) if k % 2 else
                         (lambda e, k=k, b=b: e.tensor_copy(out=yT[:, k, :], in_=psb[:, 1024 * b:1024 * b + 512])),
                         r=[PS(b)], w=['yT'])
                for t in range(4):
                    for hf in range(2):
                        b = 4 + pb[0] % 4
                        pb[0] += 1
                        for k in range(12):
                            P.op('pe', lambda e, k=k, t=t, hf=hf, b=b: e.matmul(
                                bank(b), lhsT=yT[:, k, 128 * t:128 * (t + 1)], rhs=Wo[:, k, 512 * hf:512 * (hf + 1)],
                                start=(k == 0), stop=(k == 11)), r=WO + ['yT'], w=[PS(b)])
                        P.op('dve', lambda e, t=t, hf=hf, b=b: e.tensor_tensor(
                            out=x1[:, t, 512 * hf:512 * (hf + 1)], in0=bank(b), in1=xin[:, t, 512 * hf:512 * (hf + 1)],
                            op=ALU.add), r=[PS(b), 'xinC'], w=['x1'])
                if not last:
                    P.dma('pool', 'xs', lambda e, tok=tok: e.dma_start(
                        out=xr_s[tok, :].rearrange("(t p) d -> p t d", p=128), in_=x1[:]),
                        r=['x1'], w=[('xr', c)])
                else:
                    for t in range(4):
                        P.op('act', lambda e, t=t: e.activation(out=sq[:], in_=x1[:, t, :], func=AF.Square),
                             r=['x1'], w=['sqC'])
                        P.op('dve', lambda e, t=t: e.reduce_sum(out=ssq[:, t:t + 1], in_=sq[:], axis=AX.X),
                             r=['sqC'], w=['ssqC'])
                    P.op('act', lambda e: e.activation(
                        out=ssq[:], in_=ssq[:], func=AF.Sqrt, bias=epsc[:, 0:1], scale=1.0 / D),
                        r=['ssqC', 'epsc'], w=['ssqC'])
                    P.op('dve', lambda e: e.reciprocal(out=rstd[:], in_=ssq[:]), r=['ssqC'], w=['rstdC'])
                    for t in range(4):
                        P.op('dve', lambda e, t=t: e.scalar_tensor_tensor(
                            out=x1[:, t, :], in0=x1[:, t, :], scalar=rstd[:, t:t + 1], in1=fg[:],
                            op0=ALU.mult, op1=ALU.mult), r=['x1', 'rstdC', 'fg'], w=['x1'], ss=True)
                    P.dma('pool', 'xs', lambda e, tok=tok: e.dma_start(
                        out=y_out[tok, :].rearrange("(t p) d -> p t d", p=128), in_=x1[:]),
                        r=['x1'], w=[('yout', c)])
    P.build()
    return nc


def host_constants():
    bf = ml_dtypes.bfloat16
    f32 = np.float32
    pos = np.arange(S, dtype=f32)
    inv = (f32(10000.0) ** (-np.arange(0, 64, 2, dtype=f32) / f32(64))).astype(f32)
    ang = (pos[:, None] * inv[None, :]).astype(f32)
    cos = np.cos(ang).astype(f32).T
    sin = np.sin(ang).astype(f32).T
    cos64 = np.concatenate([cos, cos], 0)
    sin64 = np.concatenate([-sin, sin], 0)
    c_cos = np.ascontiguousarray(np.concatenate([cos64, cos64], 0))
    c_sin = np.ascontiguousarray(np.concatenate([sin64, sin64], 0))
    kk = np.arange(128)[:, None]
    qq = np.arange(128)[None, :]
    tri = np.where(qq >= kk, 0.0, NEG).astype(bf)

    def split3(v):
        v = v.astype(f32)
        a = v.astype(bf)
        r = (v - a.astype(f32)).astype(f32)
        b = r.astype(bf)
        r2 = (r - b.astype(f32)).astype(f32)
        return np.stack([a, b, r2.astype(bf)], axis=-2)

    slopes = (f32(2.0) ** (-f32(8.0) * np.arange(1, 9, dtype=f32) / f32(8))).astype(f32)
    mp = (slopes[:, None] * pos[None, :]).astype(f32)
    ones = np.ones((8, 3, S), dtype=bf)
    c_mq = np.concatenate([split3(-mp), ones], axis=1)
    onehot = (np.arange(16)[:, None] == (np.arange(S)[None, :] // 256)).astype(f32)
    c_mk = np.concatenate([np.broadcast_to(onehot[None].astype(bf), (8, 16, S)), ones, split3(mp)], axis=1)
    return dict(c_ident=np.eye(128, dtype=f32), c_tri=tri, c_cos=c_cos, c_sin=c_sin,
                c_ones=ones, c_mq=np.ascontiguousarray(c_mq.astype(bf)),
                c_mk=np.ascontiguousarray(c_mk.astype(bf)))


def host_layout(inputs):
    f32 = np.float32
    w_in = np.asarray(inputs["w_in"], dtype=f32)
    kr = w_in[:, :, C_KR:C_KR + 64]
    krs = np.concatenate([kr[:, :, 32:], kr[:, :, :32]], axis=-1)
    w_in_x = np.ascontiguousarray(np.concatenate([w_in, krs], axis=-1))
    w_uq = np.asarray(inputs["mla_w_uq"], dtype=f32).reshape(2, 256, 4, 192)
    nope = w_uq[..., :128].reshape(2, 256, 512)
    rope = w_uq[..., 128:]
    ropes = np.concatenate([rope[..., 32:], rope[..., :32]], axis=-1)
    w_uq_x = np.ascontiguousarray(np.concatenate(
        [nope, rope.reshape(2, 256, 256), ropes.reshape(2, 256, 256)], axis=-1))
    w_ukv = np.asarray(inputs["mla_w_ukv"], dtype=f32).reshape(2, 128, 4, 256)
    w_ukv_x = np.ascontiguousarray(np.concatenate(
        [w_ukv[..., :128].reshape(2, 128, 512), w_ukv[..., 128:].reshape(2, 128, 512)], axis=-1))
    common = dict(
        ln_g=np.ascontiguousarray(np.asarray(inputs["ln_g"], f32).reshape(2, 8, 128).transpose(0, 2, 1)),
        w_in=w_in_x,
        fox_b_f=np.asarray(inputs["fox_b_f"], f32).reshape(2, 8, 1),
        mla_q_g=np.ascontiguousarray(np.asarray(inputs["mla_q_g"], f32).reshape(2, 2, 128).transpose(0, 2, 1)),
        mla_w_uq=w_uq_x, mla_kv_g=np.asarray(inputs["mla_kv_g"], f32).reshape(2, 128, 1),
        mla_w_ukv=w_ukv_x,
        out_g=np.ascontiguousarray(np.asarray(inputs["out_g"], f32).reshape(2, 12, 128).transpose(0, 2, 1)),
        w_out=np.asarray(inputs["w_out"], f32),
        final_g=np.asarray(inputs["final_g"], f32))
    common.update(host_constants())
    return common


_NC_CACHE = {}


def kernel(**inputs):
    x = np.asarray(inputs["x"], dtype=np.float32)
    common = host_layout(inputs)
    if 'nc' not in _NC_CACHE:
        _NC_CACHE['nc'] = build_program()
    nc = _NC_CACHE['nc']
    in_maps = [dict(common, x=np.ascontiguousarray(x[b])) for b in range(8)]
    res = run_bass_kernel_spmd(nc, in_maps, core_ids=list(range(8)))
    return np.stack([np.asarray(r["y"], dtype=np.float32) for r in res.results], axis=0)
```

```python
import numpy as np
import ml_dtypes
import concourse.bass as bass
import concourse.mybir as mybir
from concourse.bass_utils import run_bass_kernel_spmd

F32 = mybir.dt.float32
BF16 = mybir.dt.bfloat16
AF = mybir.ActivationFunctionType
ALU = mybir.AluOpType
AX = mybir.AxisListType

S = 4096
D = 1024
NCH = 8
NT = 32
DIN = 5064
DINX = 5128
EPS = 1e-6
NEG = -30000.0
C_FQ, C_FK, C_FV, C_FF, C_FZ = 0, 512, 1024, 1536, 1544
C_CQ, C_CKV, C_KR, C_MZ = 2056, 2312, 2440, 2504
C_BQ, C_BK, C_BV, C_BZ = 3016, 3528, 4040, 4552
C_KRS = 5064

ENGS = ('pe', 'act', 'dve', 'pool', 'sp')
SKIP = set()
NCH_RUN = [8]
SAME_ENG_SYNC = False


class _Rec:
    def __init__(self):
        self.call = None

    def __getattr__(self, name):
        def f(*a, **kw):
            self.call = (name, a, kw)
            return self
        return f


def _capture(fn):
    r = _Rec()
    fn(r)
    name, a, kw = r.call
    return lambda eng: getattr(eng, name)(*a, **kw)


class Prog:
    def __init__(self, nc):
        self.nc = nc
        self.ops = {e: [] for e in ENGS}
        self.last_w = {}
        self.readers = {}
        self.dma_n = {}
        self.base = set()
        self.skip = False

    def _deps(self, r, w):
        d = set(self.base)
        for x in r:
            ev = self.last_w.get(x)
            if ev is not None:
                d.add(ev)
        for x in w:
            ev = self.last_w.get(x)
            if ev is not None:
                d.add(ev)
            for ev in self.readers.get(x, {}).values():
                d.add(ev)
        return d

    def _commit(self, ev, r, w):
        for x in r:
            self.readers.setdefault(x, {})[ev[:2]] = ev
        for x in w:
            self.last_w[x] = ev
            self.readers[x] = {}

    def sec(self, name):
        self.skip = name in SKIP

    def op(self, eng, fn, r=(), w=(), ss=False):
        if self.skip:
            return
        idx = len(self.ops[eng])
        ev = ('e', eng, idx)
        deps = self._deps(r, w)
        self.ops[eng].append(dict(fn=_capture(fn), deps=deps, dma=None, ss=ss))
        self._commit(ev, r, w)

    def dma(self, eng, sem, fn, r=(), w=()):
        if self.skip:
            return
        n = self.dma_n.get(sem, 0) + 1
        self.dma_n[sem] = n
        ev = ('d', sem, n)
        deps = self._deps(r, w)
        if n > 1:
            deps.add(('d', sem, n - 1))
        self.ops[eng].append(dict(fn=_capture(fn), deps=deps, dma=sem))
        self._commit(ev, r, w)

    def barrier(self):
        b = set()
        for e in ENGS:
            for idx in range(len(self.ops[e]) - 1, -1, -1):
                if self.ops[e][idx]['dma'] is None:
                    b.add(('e', e, idx))
                    break
        for sem, n in self.dma_n.items():
            b.add(('d', sem, n))
        self.base = b

    def build(self):
        nc = self.nc
        ops = self.ops
        dma_n = self.dma_n
        plan = {e: [] for e in ENGS}
        sig = {e: set() for e in ENGS}
        for e in ENGS:
            waited = {}
            for o in ops[e]:
                need = {}
                for (kind, key, val) in o['deps']:
                    if kind == 'e' and key == e and (e == 'pe' or not (SAME_ENG_SYNC or o.get('ss'))):
                        continue
                    k2 = (kind, key)
                    if waited.get(k2, -1) >= val:
                        continue
                    need[k2] = max(need.get(k2, -1), val)
                for k2, v in need.items():
                    waited[k2] = v
                    if k2[0] == 'e':
                        sig[k2[1]].add(v)
                plan[e].append(need)
        cnt = {e: {idx: i + 1 for i, idx in enumerate(sorted(sig[e]))} for e in ENGS}
        sems = {e: nc.alloc_semaphore('s_' + e) for e in ENGS}
        dsems = {k: nc.alloc_semaphore('d_' + k) for k in self.dma_n}

        def replay(e, eng):
            lastd = {}
            for idx, o in enumerate(ops[e]):
                for k2, v in plan[e][idx].items():
                    if k2[0] == 'e':
                        eng.wait_ge(sems[k2[1]], cnt[k2[1]][v])
                    else:
                        eng.wait_ge(dsems[k2[1]], 16 * v)
                        lastd[k2[1]] = v
                ins = o['fn'](eng)
                if o['dma'] is not None:
                    ins.then_inc(dsems[o['dma']], 16)
                elif idx in cnt[e]:
                    ins.then_inc(sems[e], 1)
            if e == 'sp':
                for k, n in dma_n.items():
                    if lastd.get(k, 0) < n:
                        eng.wait_ge(dsems[k], 16 * n)

        with nc.Block() as block:
            @block.tensor
            def _(eng):
                replay('pe', eng)

            @block.scalar
            def _(eng):
                replay('act', eng)

            @block.vector
            def _(eng):
                replay('dve', eng)

            @block.gpsimd
            def _(eng):
                replay('pool', eng)

            @block.sync
            def _(eng):
                replay('sp', eng)


class Arena:
    def __init__(self, nc, lo, hi):
        self.nc, self.lo, self.hi, self.cur = nc, lo, hi, lo
        self.n = 0

    def alloc(self, name, shape, dtype):
        nbytes = int(np.prod(shape[1:])) * (4 if dtype == F32 else 2)
        nbytes = (nbytes + 63) // 64 * 64
        off = self.cur
        assert off + nbytes <= self.hi, (name, off, nbytes, self.hi)
        self.cur += nbytes
        self.n += 1
        return self.nc.alloc_sbuf_tensor_at(name, list(shape), dtype, offset=off)


def build_program(n_layers=2, dbg=None, phases=('A', 'B', 'C')):
    nc = bass.Bass("TRN2", target_bir_lowering=False)
    P = Prog(nc)
    dbg = dbg or ()

    def dt_in(name, shape, dtype=F32):
        return nc.dram_tensor(name, list(shape), dtype, kind="ExternalInput")

    def dt_scr(name, shape, dtype):
        kind = "ExternalOutput" if name in dbg else "Internal"
        return nc.dram_tensor(name, list(shape), dtype, kind=kind)

    x_in = dt_in("x", [S, D])
    ln_g = dt_in("ln_g", [2, 128, 8])
    w_in = dt_in("w_in", [2, D, DINX])
    b_f = dt_in("fox_b_f", [2, 8, 1])
    q_g = dt_in("mla_q_g", [2, 128, 2])
    w_uq = dt_in("mla_w_uq", [2, 256, 1024])
    kv_g = dt_in("mla_kv_g", [2, 128, 1])
    w_ukv = dt_in("mla_w_ukv", [2, 128, 1024])
    out_g = dt_in("out_g", [2, 128, 12])
    w_out = dt_in("w_out", [2, 1536, D])
    fin_g = dt_in("final_g", [D])
    c_ident = dt_in("c_ident", [128, 128])
    c_tri = dt_in("c_tri", [128, 128], BF16)
    c_cos = dt_in("c_cos", [128, S])
    c_sin = dt_in("c_sin", [128, S])
    c_ones = dt_in("c_ones", [8, 3, S], BF16)
    c_mq = dt_in("c_mq", [8, 6, S], BF16)
    c_mk = dt_in("c_mk", [8, 22, S], BF16)
    y_out = nc.dram_tensor("y", [S, D], F32, kind="ExternalOutput")

    fq_s = dt_scr("fq_s", [8, 70, S], BF16)
    fk_s = dt_scr("fk_s", [8, 70, S], BF16)
    fv_s = dt_scr("fv_s", [S, 8 * 65], BF16)
    bq_s = dt_scr("bq_s", [8, 86, S], BF16)
    bk_s = dt_scr("bk_s", [8, 86, S], BF16)
    bv_s = dt_scr("bv_s", [S, 8 * 65], BF16)
    mq1_s = dt_scr("mq1_s", [4, 128, S], BF16)
    mq2_s = dt_scr("mq2_s", [4, 64, S], BF16)
    mk1_s = dt_scr("mk1_s", [4, 128, S], BF16)
    mk2_s = dt_scr("mk2_s", [64, S], BF16)
    mv_s = dt_scr("mv_s", [S, 4 * 129], BF16)
    gz_s = dt_scr("gz_s", [S, 1536], F32)
    yy_s = dt_scr("yy_s", [S, 1536], BF16)
    xr_s = dt_scr("xr_s", [S, D], F32)

    LO, HI = 16512 + 64, 229344
    AP_ = Arena(nc, LO, HI)
    ident = AP_.alloc("ident", [128, 128], F32)
    identb = AP_.alloc("identb", [128, 128], BF16)
    tri = AP_.alloc("tri", [128, 128], BF16)
    onesf = AP_.alloc("onesf", [128, 128], F32)
    epsc = AP_.alloc("epsc", [128, 1], F32)
    kmT = AP_.alloc("kmT", [128, 4, 2, 16], F32)
    persist_end = AP_.cur
    WH = DINX // 2
    WA = Arena(nc, persist_end, HI)
    Wb = WA.alloc("Wb", [128, 8, DINX], BF16)
    wst = WA.alloc("wst", [128, WH], F32)
    gcol = WA.alloc("gcol", [128, 8], F32)
    wend = WA.cur
    TOPLO = HI - (12 * D * 2 + D * 4 + 64 + 64)
    TA = Arena(nc, TOPLO, HI)
    Wo = TA.alloc("Wo", [128, 12, D], BF16)
    wstC = TA.alloc("wstC", [128, D], F32)
    ogc = TA.alloc("ogc", [128, 12], F32)
    WB_K = lambda k: [('Wb', k, 0), ('Wb', k, 1)]

    def load_win(l_, pieces, stg):
        for n_, (k, hf) in enumerate(pieces):
            st_, rs_, sm_ = stg[n_ % len(stg)]
            P.dma('sp', sm_, lambda e: e.dma_start(
                out=st_[:, 0:WH], in_=w_in[l_, 128 * k:128 * (k + 1), WH * hf:WH * (hf + 1)]), w=[rs_])
            if hf:
                P.op('act', lambda e: e.activation(
                    out=Wb[:, k, WH * hf:WH * (hf + 1)], in_=st_[:, 0:WH], func=AF.Copy, scale=gcol[:, k:k + 1]),
                    r=[rs_, 'gcol'], w=[('Wb', k, hf)])
            else:
                P.op('dve', lambda e: e.tensor_scalar(
                    out=Wb[:, k, WH * hf:WH * (hf + 1)], in0=st_[:, 0:WH], scalar1=gcol[:, k:k + 1], scalar2=None,
                    op0=ALU.mult), r=[rs_, 'gcol'], w=[('Wb', k, hf)])

    def load_wout(l_, ks):
        for k in ks:
            if k == 0:
                P.dma('sp', 'wgo', lambda e: e.dma_start(out=ogc[:], in_=out_g[l_, :, :]), w=['ogc'])
            P.dma('sp', 'wstC', lambda e: e.dma_start(
                out=wstC[:], in_=w_out[l_, 128 * k:128 * (k + 1), :]), w=['wstC'])
            P.op('dve', lambda e: e.tensor_scalar(
                out=Wo[:, k, :], in0=wstC[:], scalar1=ogc[:, k:k + 1], scalar2=None, op0=ALU.mult),
                r=['wstC', 'ogc'], w=[('Wo', k)])

    ALLW = [(k, hf) for k in range(8) for hf in range(2)]
    ps = nc.alloc_psum_tensor("ps", [128, 4096], F32)
    psb = ps.bitcast(BF16)

    def bank(b, lo=0, hi=512):
        return ps[:, 512 * b + lo: 512 * b + hi]

    def PS(b):
        return ('ps', b)

    P.sec('init')
    P.dma('sp', 'c0', lambda e: e.dma_start(out=ident[:], in_=c_ident[:, :]), w=['ident'])
    P.dma('sp', 'c1', lambda e: e.dma_start(out=tri[:], in_=c_tri[:, :]), w=['tri'])
    P.op('dve', lambda e: e.tensor_copy(out=identb[:], in_=ident[:]), r=['ident'], w=['identb'])
    P.op('dve', lambda e: e.memset(onesf[:], 1.0), w=['onesf'])
    P.op('dve', lambda e: e.memset(epsc[:], EPS), w=['epsc'])
    P.op('dve', lambda e: e.memset(kmT[:], 0.0), w=[('kmT', j) for j in range(4)])
    P.dma('sp', 'c2', lambda e: e.dma_start(out=fq_s[:, 67:70, :], in_=c_ones[:, :, :]), w=[('fq', 'const')])
    P.dma('sp', 'c3', lambda e: e.dma_start(out=fk_s[:, 64:67, :], in_=c_ones[:, :, :]), w=[('fk', 'const')])
    P.dma('sp', 'c4', lambda e: e.dma_start(out=bq_s[:, 80:86, :], in_=c_mq[:, :, :]), w=[('bq', 'const')])
    P.dma('sp', 'c5', lambda e: e.dma_start(out=bk_s[:, 64:86, :], in_=c_mk[:, :, :]), w=[('bk', 'const')])

    for l in range(n_layers):
        x_src = x_in if l == 0 else xr_s
        last = (l == n_layers - 1)
        if 'A' in phases:
            P.barrier()
            A = Arena(nc, wend, HI)
            Wuq = A.alloc("Wuq", [128, 2, 1024], BF16)
            Wukv = A.alloc("Wukv", [128, 1024], BF16)
            gq = A.alloc("gq", [128, 2], F32)
            gkv = A.alloc("gkv", [128, 1], F32)
            bfc = A.alloc("bfc", [8, 1], F32)
            xin_off = A.cur
            xin = [A.alloc("xin0", [128, 4, D], F32)] * 2
            xT = A.alloc("xT", [128, 8, 512], BF16)
            sq = A.alloc("sq", [128, D], F32)
            ssqs = [A.alloc(f"ssq{i}", [128, 4], F32) for i in range(2)]
            rstds = [A.alloc(f"rstd{i}", [128, 4], F32) for i in range(2)]
            Rt = A.alloc("Rt", [128, 4, 128], F32)
            rbc = A.alloc("rbc", [128, 512], F32)
            fmst = [A.alloc(f"fmst{i}", [128, 512], BF16) for i in range(4)]
            qf = A.alloc("qf", [128, 4, 512], F32)
            kf = A.alloc("kf", [128, 512], F32)
            cqT = A.alloc("cqT", [128, 2, 512], F32)
            sq2 = A.alloc("sq2", [128, 2, 512], F32)
            rq = A.alloc("rq", [128, 512], F32)
            cqn = A.alloc("cqn", [128, 2, 512], BF16)
            ckvn = A.alloc("ckvn", [128, 512], BF16)
            cosc = A.alloc("cosc", [128, 512], F32)
            sinc = A.alloc("sinc", [128, 512], F32)
            t1 = A.alloc("t1", [128, 512], F32)
            t2 = A.alloc("t2", [128, 512], F32)
            ffx = A.alloc("ffx", [8, 512], F32)
            ffe = A.alloc("ffe", [8, 512], F32)
            ones8 = A.alloc("ones8", [8, 512], F32)
            cc = [A.alloc(f"cc{i}", [8, 512], F32) for i in range(2)]
            r1 = A.alloc("r1", [8, 512], F32)
            cs = A.alloc("cs", [8, 3, 512], BF16)
            ncs = A.alloc("ncs", [8, 3, 512], BF16)
            vst = A.alloc("vst", [128, 4, 8 * 65], BF16)
            vst2 = A.alloc("vst2", [128, 4, 8 * 65], BF16)
            mvst = A.alloc("mvst", [128, 4, 4 * 129], BF16)
            gzst = A.alloc("gzst", [128, 1536], F32)
            gate = A.alloc("gate", [128, 8, 16], F32)
            mx8 = A.alloc("mx8", [128, 8, 8], F32)
            masks = [A.alloc(f"mask{i}", [128, 8, 16], BF16) for i in range(4)]
            maskT = A.alloc("maskT", [16, 8, 128], BF16)

            P.sec('weights')
            if l == 0:
                wst2 = nc.alloc_sbuf_tensor_at("wst2", [128, WH], F32, offset=xin_off)
                P.dma('sp', 'wg', lambda e: e.dma_start(out=gcol[:], in_=ln_g[l, :, :]), w=['gcol'])
                load_win(0, ALLW, [(wst, 'wst', 'wst'), (wst2, ('xin', 0), 'xin0')])
            WB = [('Wb', k, j) for k in range(8) for j in range(2)]
            P.dma('sp', 'wg2', lambda e: e.dma_start(
                out=gq[:], in_=q_g[l, :, :]), w=['gq'])
            P.dma('sp', 'wg3', lambda e: e.dma_start(
                out=gkv[:], in_=kv_g[l, :, :]), w=['gkv'])
            P.dma('sp', 'wg4', lambda e: e.dma_start(
                out=bfc[:], in_=b_f[l, :, :]), w=['bfc'])
            for k in range(2):
                P.dma('sp', 'wst', lambda e, k=k: e.dma_start(
                    out=wst[:, 0:1024], in_=w_uq[l, 128 * k:128 * (k + 1), :]), w=['wst'])
                P.op('dve', lambda e, k=k: e.tensor_scalar(
                    out=Wuq[:, k, :], in0=wst[:, 0:1024], scalar1=gq[:, k:k + 1], scalar2=None,
                    op0=ALU.mult), r=['wst', 'gq'], w=['Wuq'])
            P.dma('sp', 'wst', lambda e: e.dma_start(
                out=wst[:, 0:1024], in_=w_ukv[l, :, :]), w=['wst'])
            P.op('dve', lambda e: e.tensor_scalar(
                out=Wukv[:], in0=wst[:, 0:1024], scalar1=gkv[:, 0:1], scalar2=None,
                op0=ALU.mult), r=['wst', 'gkv'], w=['Wukv'])
            P.op('dve', lambda e: e.memset(ones8[:], 1.0), w=['ones8'])
            P.op('dve', lambda e: e.memset(cc[1][:], 0.0), w=[('cc', 1)])
            P.op('pool', lambda e: e.memset(vst[:], 1.0), w=['vst'])
            P.op('pool', lambda e: e.memset(vst2[:], 1.0), w=['vst2'])
            P.op('pool', lambda e: e.memset(mvst[:], 1.0), w=['mvst'])

            pb = [0]

            def nb():
                pb[0] = (pb[0] + 1) % 8
                return pb[0]

            def store_pair(st, si, dst, j, tok):
                for hh in range(2):
                    P.dma('sp', f'fm{si}_{hh}', lambda e, hh=hh: e.dma_start(
                        out=dst[2 * j + hh, 0:64, tok], in_=st[64 * hh:64 * hh + 64, :]),
                        r=[('fmst', si)])

            def fm_job(col, ncols, rows_lo=0):
                b = nb()
                for k in range(8):
                    P.op('pe', lambda e, k=k, b=b: e.matmul(
                        bank(b)[rows_lo:rows_lo + ncols, :], lhsT=Wb[:, k, col:col + ncols],
                        rhs=xT[:, k, :], start=(k == 0), stop=(k == 7)),
                        r=WB_K(k) + ['xT'], w=[PS(b)])
                return b

            XI = ('xin', 0)
            xi = xin[0]
            qs = 192.0 ** -0.5

            def tokc(c):
                return slice(512 * c, 512 * (c + 1))

            def load_x(c):
                P.sec('pre')
                P.dma('sp', 'xin0', lambda e: e.dma_start(
                    out=xi[:], in_=x_src[tokc(c), :].rearrange("(t p) d -> p t d", p=128)), w=[XI])

            def load_trig(c):
                P.sec('mla')
                P.dma('sp', 'cosd', lambda e: e.dma_start(out=cosc[:], in_=c_cos[:, tokc(c)]), w=['cosc'])
                P.dma('sp', 'sind', lambda e: e.dma_start(out=sinc[:], in_=c_sin[:, tokc(c)]), w=['sinc'])

            def pre_stats(c):
                P.sec('pre')
                ssq, rstd = ssqs[c % 2], rstds[c % 2]
                SSQ, RSTD = ('ssq', c % 2), ('rstd', c % 2)
                for t in range(4):
                    P.op('dve', lambda e: e.tensor_tensor(
                        out=sq[:], in0=xi[:, t, :], in1=xi[:, t, :], op=ALU.mult), r=[XI], w=['sq'])
                    P.op('dve', lambda e: e.reduce_sum(
                        out=ssq[:, t:t + 1], in_=sq[:], axis=AX.X), r=['sq'], w=[SSQ])
                P.op('act', lambda e: e.activation(
                    out=ssq[:], in_=ssq[:], func=AF.Sqrt, bias=epsc[:, 0:1], scale=1.0 / D),
                    r=[SSQ, 'epsc'], w=[SSQ])
                P.op('dve', lambda e: e.reciprocal(out=rstd[:], in_=ssq[:]), r=[SSQ], w=[RSTD])
                for t in range(4):
                    P.op('dve', lambda e: e.tensor_scalar(
                        out=Rt[:, t, :], in0=onesf[:], scalar1=rstd[:, t:t + 1], scalar2=None, op0=ALU.mult),
                        r=['onesf', RSTD], w=[('Rt', t)], ss=True)

            def pre_pe(c):
                P.sec('pre')
                for k in range(8):
                    b = nb()
                    for t in range(4):
                        P.op('pe', lambda e: e.transpose(
                            out=bank(b, 128 * t, 128 * (t + 1)), in_=xi[:, t, 128 * k:128 * (k + 1)],
                            identity=ident[:]), r=[XI, 'ident'], w=[PS(b)])
                    if k % 2 == 0:
                        P.op('act', lambda e: e.copy(out=xT[:, k, :], in_=bank(b)), r=[PS(b)], w=['xT'])
                    else:
                        P.op('dve', lambda e: e.tensor_copy(out=xT[:, k, :], in_=bank(b)), r=[PS(b)], w=['xT'])
                b = nb()
                for t in range(4):
                    P.op('pe', lambda e: e.matmul(
                        bank(b, 128 * t, 128 * (t + 1)), lhsT=Rt[:, t, :], rhs=ident[:], start=True, stop=True),
                        r=[('Rt', t), 'ident'], w=[PS(b)])
                P.op('act', lambda e: e.copy(out=rbc[:], in_=bank(b)), r=[PS(b)], w=['rbc'])

            def s_fox(c):
                P.sec('fox')
                tok = tokc(c)
                for j in range(4):
                    b = fm_job(C_FQ + 128 * j, 128)
                    st = fmst[j % 4]
                    P.op('dve', lambda e: e.scalar_tensor_tensor(
                        out=st[:], in0=bank(b), scalar=0.125, in1=rbc[:], op0=ALU.mult, op1=ALU.mult),
                        r=[PS(b), 'rbc'], w=[('fmst', j % 4)])
                    store_pair(st, j % 4, fq_s, j, tok)
                for j in range(4):
                    b = fm_job(C_FK + 128 * j, 128)
                    st = fmst[j % 4]
                    P.op('dve', lambda e: e.tensor_tensor(
                        out=st[:], in0=bank(b), in1=rbc[:], op=ALU.mult),
                        r=[PS(b), 'rbc'], w=[('fmst', j % 4)])
                    store_pair(st, j % 4, fk_s, j, tok)

            def s_ff(c):
                P.sec('ff')
                tok = tokc(c)
                b = fm_job(C_FF, 8)
                P.op('dve', lambda e: e.tensor_tensor(
                    out=ffx[:], in0=bank(b)[0:8, :], in1=rbc[0:8, :], op=ALU.mult),
                    r=[PS(b), 'rbc'], w=['ffx'])
                P.op('dve', lambda e: e.tensor_scalar(
                    out=ffx[:], in0=ffx[:], scalar1=bfc[:, 0:1], scalar2=-1.0, op0=ALU.add, op1=ALU.mult),
                    r=['ffx', 'bfc'], w=['ffx'])
                P.op('act', lambda e: e.activation(out=ffe[:], in_=ffx[:], func=AF.Exp), r=['ffx'], w=['ffe'])
                P.op('act', lambda e: e.activation(out=ffe[:], in_=ffe[:], func=AF.Ln, bias=onesf[0:8, 0:1]),
                     r=['ffe', 'onesf'], w=['ffe'], ss=True)

            def s_ff_b(c):
                P.sec('ff')
                tok = tokc(c)
                cprev, ccur = cc[(c + 1) % 2], cc[c % 2]
                if c == 0:
                    P.op('dve', lambda e: e.tensor_tensor_scan(
                        out=ccur[:], data0=ones8[:], data1=ffe[:], initial=0.0,
                        op0=ALU.mult, op1=ALU.subtract), r=['ones8', 'ffe'], w=[('cc', c % 2)])
                else:
                    P.op('dve', lambda e: e.tensor_tensor_scan(
                        out=ccur[:], data0=ones8[:], data1=ffe[:], initial=cprev[:, 511:512],
                        op0=ALU.mult, op1=ALU.subtract),
                        r=['ones8', 'ffe', ('cc', (c + 1) % 2)], w=[('cc', c % 2)], ss=True)
                P.op('dve', lambda e: e.tensor_copy(out=cs[:, 0, :], in_=ccur[:]), r=[('cc', c % 2)], w=['cs'])
                P.op('dve', lambda e: e.tensor_tensor(
                    out=r1[:], in0=ccur[:], in1=cs[:, 0, :], op=ALU.subtract),
                    r=[('cc', c % 2), 'cs'], w=['r1'])
                P.op('dve', lambda e: e.tensor_copy(out=cs[:, 1, :], in_=r1[:]), r=['r1'], w=['cs'])
                P.op('dve', lambda e: e.tensor_tensor(
                    out=r1[:], in0=r1[:], in1=cs[:, 1, :], op=ALU.subtract), r=['r1', 'cs'], w=['r1'])
                P.op('dve', lambda e: e.tensor_copy(out=cs[:, 2, :], in_=r1[:]), r=['r1'], w=['cs'])
                P.op('dve', lambda e: e.tensor_scalar(
                    out=ncs[:], in0=cs[:], scalar1=-1.0, scalar2=None, op0=ALU.mult), r=['cs'], w=['ncs'])
                P.dma('sp', 'csd', lambda e: e.dma_start(out=fq_s[:, 64:67, tok], in_=cs[:]), r=['cs'])
                P.dma('sp', 'ncsd', lambda e: e.dma_start(out=fk_s[:, 67:70, tok], in_=ncs[:]), r=['ncs'])

            def s_moba(c):
                P.sec('moba')
                tok = tokc(c)
                for j in range(4):
                    b = fm_job(C_BQ + 128 * j, 128)
                    st = fmst[j % 4]
                    P.op('dve', lambda e: e.scalar_tensor_tensor(
                        out=qf[:, j, :], in0=bank(b), scalar=0.125, in1=rbc[:], op0=ALU.mult, op1=ALU.mult),
                        r=[PS(b), 'rbc'], w=[('qf', j)])
                    P.op('pool', lambda e: e.tensor_copy(out=st[:], in_=qf[:, j, :]),
                         r=[('qf', j)], w=[('fmst', j % 4)])
                    store_pair(st, j % 4, bq_s, j, tok)
                for j in range(4):
                    b = fm_job(C_BK + 128 * j, 128)
                    st = fmst[j % 4]
                    P.op('dve', lambda e: e.tensor_tensor(
                        out=kf[:], in0=bank(b), in1=rbc[:], op=ALU.mult), r=[PS(b), 'rbc'], w=['kf'])
                    for hh in range(2):
                        P.op('dve', lambda e: e.reduce_sum(
                            out=kmT[64 * hh:64 * hh + 64, j, hh, 2 * c:2 * c + 2],
                            in_=kf[64 * hh:64 * hh + 64, :].rearrange("p (n s) -> p n s", s=256),
                            axis=AX.X), r=['kf'], w=[('kmT', j)])
                    P.op('pool', lambda e: e.tensor_copy(out=st[:], in_=kf[:]),
                         r=['kf'], w=[('fmst', j % 4)])
                    store_pair(st, j % 4, bk_s, j, tok)

            gate_bank = [0]

            def s_gate_mm(c):
                P.sec('gate')
                b = nb()
                gate_bank[0] = b
                for t in range(4):
                    for j in range(4):
                        P.op('pe', lambda e: e.matmul(
                            bank(b, 128 * t + 32 * j, 128 * t + 32 * j + 32),
                            lhsT=qf[:, j, 128 * t:128 * (t + 1)],
                            rhs=kmT[:, j, :, :].rearrange("p a n -> p (a n)"), start=True, stop=True),
                            r=[('qf', j), ('kmT', j)], w=[PS(b)])

            def s_gate_dve(c):
                P.sec('gate_top')
                b = gate_bank[0]
                for t in range(4):
                    cur = 2 * c + t // 2
                    mk = masks[t]
                    MK = ('mask', t)
                    P.op('dve', lambda e: e.memset(gate[:], NEG), w=['gate'])
                    if cur > 0:
                        P.op('dve', lambda e: e.tensor_copy(
                            out=gate[:, :, 0:cur],
                            in_=bank(b, 128 * t, 128 * (t + 1)).rearrange("p (h n) -> p h n", n=16)[:, :, 0:cur]),
                            r=[PS(b)], w=['gate'])
                    for h in range(8):
                        P.op('dve', lambda e: e.max(out=mx8[:, h, :], in_=gate[:, h, :]), r=['gate'], w=['mx8'])
                    for h in range(8):
                        P.op('dve', lambda e: e.tensor_scalar(
                            out=mk[:, h, :], in0=gate[:, h, :], scalar1=mx8[:, h, 2:3], scalar2=NEG,
                            op0=ALU.is_lt, op1=ALU.mult), r=['gate', 'mx8'], w=[MK], ss=True)
                    if cur <= 3 and cur > 0:
                        P.op('dve', lambda e: e.memset(mk[:, :, 0:cur], 0.0), w=[MK])
                    P.op('dve', lambda e: e.memset(mk[:, :, cur:cur + 1], 0.0), w=[MK])
                    if cur < 15:
                        P.op('dve', lambda e: e.memset(mk[:, :, cur + 1:16], NEG), w=[MK])

            def s_gate_tr(c):
                P.sec('gate_tr')
                for t in range(4):
                    mk = masks[t]
                    b2 = nb()
                    for h in range(8):
                        P.op('pe', lambda e: e.transpose(
                            out=psb[0:16, 1024 * b2 + 128 * h: 1024 * b2 + 128 * (h + 1)],
                            in_=mk[:, h, :], identity=identb[:]),
                            r=[('mask', t), 'identb'], w=[PS(b2)])
                    P.op('act', lambda e: e.copy(
                        out=maskT[:],
                        in_=psb[0:16, 1024 * b2:1024 * b2 + 1024].rearrange("p (h q) -> p h q", q=128)),
                        r=[PS(b2)], w=['maskT'])
                    P.dma('sp', 'mskd', lambda e: e.dma_start(
                        out=bq_s[:, 64:80, 512 * c + 128 * t:512 * c + 128 * (t + 1)].rearrange("h n s -> n h s"),
                        in_=maskT[:]), r=['maskT'])

            def s_mla_q_fm(c):
                P.sec('mla')
                for j in range(2):
                    b = fm_job(C_CQ + 128 * j, 128)
                    P.op('dve', lambda e: e.tensor_tensor(
                        out=cqT[:, j, :], in0=bank(b), in1=rbc[:], op=ALU.mult),
                        r=[PS(b), 'rbc'], w=[('cqT', j)])
                    P.op('act', lambda e: e.activation(out=sq2[:, j, :], in_=cqT[:, j, :], func=AF.Square),
                         r=[('cqT', j)], w=[('sq2', j)])

            def s_mla_q_norm(c):
                P.sec('mla')
                b = nb()
                for j in range(2):
                    P.op('pe', lambda e: e.matmul(
                        bank(b), lhsT=onesf[:], rhs=sq2[:, j, :], start=(j == 0), stop=(j == 1)),
                        r=['onesf', ('sq2', j)], w=[PS(b)])
                P.op('act', lambda e: e.activation(
                    out=rq[:], in_=bank(b), func=AF.Sqrt, bias=epsc[:, 0:1], scale=1.0 / 256),
                    r=[PS(b), 'epsc'], w=['rq'])
                P.op('dve', lambda e: e.reciprocal(out=rq[:], in_=rq[:]), r=['rq'], w=['rq'])
                for j in range(2):
                    P.op('dve', lambda e: e.tensor_tensor(
                        out=cqn[:, j, :], in0=cqT[:, j, :], in1=rq[:], op=ALU.mult),
                        r=[('cqT', j), 'rq'], w=['cqn'])

            def s_mla_kv_fm(c):
                P.sec('mla')
                tok = tokc(c)
                b = fm_job(C_CKV, 128)
                P.op('dve', lambda e: e.tensor_tensor(
                    out=cqT[:, 0, :], in0=bank(b), in1=rbc[:], op=ALU.mult), r=[PS(b), 'rbc'], w=[('cqT', 0)])
                P.op('act', lambda e: e.activation(out=sq2[:, 0, :], in_=cqT[:, 0, :], func=AF.Square),
                     r=[('cqT', 0)], w=[('sq2', 0)])
                bA = fm_job(C_KR, 64)
                bB = fm_job(C_KRS, 64)
                P.op('dve', lambda e: e.tensor_tensor(
                    out=t1[0:64, :], in0=bank(bA)[0:64, :], in1=rbc[0:64, :], op=ALU.mult),
                    r=[PS(bA), 'rbc'], w=['t1'])
                P.op('dve', lambda e: e.tensor_tensor(
                    out=t2[0:64, :], in0=bank(bB)[0:64, :], in1=rbc[0:64, :], op=ALU.mult),
                    r=[PS(bB), 'rbc'], w=['t2'])
                P.op('pool', lambda e: e.tensor_tensor(
                    out=t1[0:64, :], in0=t1[0:64, :], in1=cosc[0:64, :], op=ALU.mult),
                    r=['t1', 'cosc'], w=['t1'])
                P.op('pool', lambda e: e.tensor_tensor(
                    out=t2[0:64, :], in0=t2[0:64, :], in1=sinc[0:64, :], op=ALU.mult),
                    r=['t2', 'sinc'], w=['t2'])
                st = fmst[2]
                P.op('pool', lambda e: e.tensor_tensor(
                    out=st[0:64, :], in0=t1[0:64, :], in1=t2[0:64, :], op=ALU.add),
                    r=['t1', 't2'], w=[('fmst', 2)])
                P.dma('sp', 'fm2', lambda e: e.dma_start(
                    out=mk2_s[:, tok], in_=st[0:64, :]), r=[('fmst', 2)])

            def s_mla_kv_norm(c):
                P.sec('mla')
                b = nb()
                P.op('pe', lambda e: e.matmul(
                    bank(b), lhsT=onesf[:], rhs=sq2[:, 0, :], start=True, stop=True),
                    r=['onesf', ('sq2', 0)], w=[PS(b)])
                P.op('act', lambda e: e.activation(
                    out=rq[:], in_=bank(b), func=AF.Sqrt, bias=epsc[:, 0:1], scale=1.0 / 128),
                    r=[PS(b), 'epsc'], w=['rq'])
                P.op('dve', lambda e: e.reciprocal(out=rq[:], in_=rq[:]), r=['rq'], w=['rq'])
                P.op('dve', lambda e: e.tensor_tensor(
                    out=ckvn[:], in0=cqT[:, 0, :], in1=rq[:], op=ALU.mult), r=[('cqT', 0), 'rq'], w=['ckvn'])

            def s_mla_q_up(c):
                P.sec('mla')
                tok = tokc(c)
                for h in range(4):
                    b = nb()
                    for k in range(2):
                        P.op('pe', lambda e: e.matmul(
                            bank(b), lhsT=Wuq[:, k, 128 * h:128 * (h + 1)], rhs=cqn[:, k, :],
                            start=(k == 0), stop=(k == 1)), r=['Wuq', 'cqn'], w=[PS(b)])
                    st = fmst[h % 4]
                    P.op('act', lambda e: e.mul(out=st[:], in_=bank(b), mul=qs),
                         r=[PS(b)], w=[('fmst', h % 4)])
                    P.dma('sp', f'fm{h % 4}', lambda e: e.dma_start(
                        out=mq1_s[h, :, tok], in_=st[:]), r=[('fmst', h % 4)])
                for p in range(2):
                    bA, bB = nb(), nb()
                    for k in range(2):
                        P.op('pe', lambda e: e.matmul(
                            bank(bA), lhsT=Wuq[:, k, 512 + 128 * p:512 + 128 * (p + 1)], rhs=cqn[:, k, :],
                            start=(k == 0), stop=(k == 1)), r=['Wuq', 'cqn'], w=[PS(bA)])
                    for k in range(2):
                        P.op('pe', lambda e: e.matmul(
                            bank(bB), lhsT=Wuq[:, k, 768 + 128 * p:768 + 128 * (p + 1)], rhs=cqn[:, k, :],
                            start=(k == 0), stop=(k == 1)), r=['Wuq', 'cqn'], w=[PS(bB)])
                    P.op('dve', lambda e: e.tensor_tensor(
                        out=t1[:], in0=bank(bA), in1=cosc[:], op=ALU.mult), r=[PS(bA), 'cosc'], w=['t1'])
                    P.op('dve', lambda e: e.tensor_tensor(
                        out=t2[:], in0=bank(bB), in1=sinc[:], op=ALU.mult), r=[PS(bB), 'sinc'], w=['t2'])
                    P.op('pool', lambda e: e.tensor_tensor(
                        out=t1[:], in0=t1[:], in1=t2[:], op=ALU.add), r=['t1', 't2'], w=['t1'])
                    st = fmst[p]
                    P.op('act', lambda e: e.mul(out=st[:], in_=t1[:], mul=qs), r=['t1'], w=[('fmst', p)])
                    store_pair(st, p, mq2_s, p, tok)

            def s_mla_kv_up(c):
                P.sec('mla')
                tok = tokc(c)
                for h in range(4):
                    b = nb()
                    P.op('pe', lambda e: e.matmul(
                        bank(b), lhsT=Wukv[:, 128 * h:128 * (h + 1)], rhs=ckvn[:], start=True, stop=True),
                        r=['Wukv', 'ckvn'], w=[PS(b)])
                    st = fmst[h % 4]
                    P.op('act', lambda e: e.copy(out=st[:], in_=bank(b)), r=[PS(b)], w=[('fmst', h % 4)])
                    P.dma('sp', f'fm{h % 4}', lambda e: e.dma_start(
                        out=mk1_s[h, :, tok], in_=st[:]), r=[('fmst', h % 4)])
                for t in range(4):
                    b = nb()
                    P.op('pe', lambda e: e.matmul(
                        bank(b), lhsT=ckvn[:, 128 * t:128 * (t + 1)], rhs=Wukv[:, 512:1024],
                        start=True, stop=True), r=['Wukv', 'ckvn'], w=[PS(b)])
                    P.op('act', lambda e: e.copy(
                        out=mvst[:, t, :].rearrange("p (h d) -> p h d", d=129)[:, :, 0:128],
                        in_=bank(b).rearrange("p (h d) -> p h d", d=128)), r=[PS(b)], w=['mvst'])
                P.dma('sp', 'mvd', lambda e: e.dma_start(
                    out=mv_s[tok, :].rearrange("(t p) f -> p t f", p=128), in_=mvst[:]), r=['mvst'])

            def s_tm(c, tiles):
                P.sec('tm')
                for t in tiles:
                    for (col, kind) in ((C_FV, 'fv'), (C_BV, 'bv'), (C_FZ, 'z0'), (C_MZ, 'z1'), (C_BZ, 'z2')):
                        b = nb()
                        for k in range(8):
                            P.op('pe', lambda e: e.matmul(
                                bank(b), lhsT=xT[:, k, 128 * t:128 * (t + 1)], rhs=Wb[:, k, col:col + 512],
                                start=(k == 0), stop=(k == 7)), r=WB_K(k) + ['xT'], w=[PS(b)])
                        if kind in ('fv', 'bv'):
                            vs = vst if kind == 'fv' else vst2
                            P.op('act', lambda e: e.activation(
                                out=vs[:, t, :].rearrange("p (h d) -> p h d", d=65)[:, :, 0:64],
                                in_=bank(b).rearrange("p (h d) -> p h d", d=64), func=AF.Copy,
                                scale=rstds[c % 2][:, t:t + 1]), r=[PS(b), ('rstd', c % 2)],
                                w=['vst' if kind == 'fv' else 'vst2'])
                        else:
                            g = int(kind[1])
                            P.op('act', lambda e: e.activation(
                                out=gzst[:, 512 * g:512 * (g + 1)], in_=bank(b), func=AF.Silu,
                                scale=rstds[c % 2][:, t:t + 1]), r=[PS(b), ('rstd', c % 2)], w=['gzst'])
                    P.dma('sp', 'gzd', lambda e: e.dma_start(
                        out=gz_s[512 * c + 128 * t:512 * c + 128 * (t + 1), :], in_=gzst[:]), r=['gzst'])

            def s_vstore(c):
                P.sec('tm')
                tok = tokc(c)
                P.dma('sp', 'fvd', lambda e: e.dma_start(
                    out=fv_s[tok, :].rearrange("(t p) f -> p t f", p=128), in_=vst[:]), r=['vst'])
                P.dma('sp', 'bvd', lambda e: e.dma_start(
                    out=bv_s[tok, :].rearrange("(t p) f -> p t f", p=128), in_=vst2[:]), r=['vst2'])

            NCR = NCH_RUN[0]
            load_x(0)
            pre_stats(0)
            pre_pe(0)
            for c in range(NCR):
                if c + 1 < NCR:
                    load_x(c + 1)
                load_trig(c)
                s_fox(c)
                s_ff(c)
                s_moba(c)
                s_ff_b(c)
                s_mla_q_fm(c)
                s_tm(c, (0,))
                s_gate_mm(c)
                s_gate_dve(c)
                s_mla_q_norm(c)
                if c + 1 < NCR:
                    pre_stats(c + 1)
                s_tm(c, (1,))
                s_mla_kv_fm(c)
                s_tm(c, (2,))
                s_mla_kv_norm(c)
                s_tm(c, (3,))
                s_vstore(c)
                if c + 1 < NCR:
                    pre_pe(c + 1)
                s_mla_q_up(c)
                s_mla_kv_up(c)
                s_gate_tr(c)

        if 'B' in phases:
            mixers = (
                dict(name='fox', H=8, dv=64, rows=[70], q=[fq_s], k=[fk_s], v=fv_s, g=0,
                     qres=lambda c: [('fq', 'const'), ('fq', c, 'c')] + [('fq', c, j) for j in range(4)],
                     kres=lambda c: [('fk', 'const'), ('fk', c, 'c')] + [('fk', c, j) for j in range(4)],
                     vres=lambda c: [('fv', c)]),
                dict(name='mla', H=4, dv=128, rows=[128, 64], q=[mq1_s, mq2_s], k=[mk1_s, mk2_s], v=mv_s, g=1,
                     qres=lambda c: [('mq1', c, h) for h in range(4)] + [('mq2', c, p) for p in range(2)],
                     kres=lambda c: [('mk1', c, h) for h in range(4)] + [('mk2', c)],
                     vres=lambda c: [('mv', c)]),
                dict(name='moba', H=8, dv=64, rows=[86], q=[bq_s], k=[bk_s], v=bv_s, g=2,
                     qres=lambda c: [('bq', 'const'), ('bq', c, 'm')] + [('bq', c, j) for j in range(4)],
                     kres=lambda c: [('bk', 'const')] + [('bk', c, j) for j in range(4)],
                     vres=lambda c: [('bv', c)]),
            )
            for mx in mixers:
                P.barrier()
                B = Arena(nc, persist_end, HI)
                H, dv, rows = mx['H'], mx['dv'], mx['rows']
                dv1 = dv + 1
                np_ = len(rows)
                nm = mx['name']
                G = mx['g']
                GB = 3 if 4 * dv1 <= 512 else 2
                kT = [B.alloc(f"kT{i}", [rows[i], H if not (nm == 'mla' and i == 1) else 1, S], BF16)
                      for i in range(np_)]
                vv = B.alloc("vv", [128, NT, H * dv1], BF16)
                qT = [[B.alloc(f"qT{i}_{bf}", [rows[i], H, 512], BF16) for i in range(np_)] for bf in range(2)]
                pT = [B.alloc(f"pT{i}", [128, GB * 512], BF16) for i in range(3)]
                oo = B.alloc("oo", [128, 4, 512], F32)
                gzt = [B.alloc(f"gzt{i}", [128, 4, 512], F32) for i in range(3)]
                rcp = B.alloc("rcp", [128, 4], F32)
                osq = B.alloc("osq", [128, 512], F32)
                oss = B.alloc("oss", [128, 4], F32)
                ors = B.alloc("ors", [128, 4], F32)
                yst = B.alloc("yst", [128, 4, 512], BF16)

                if GB == 3:
                    def acc(a_, u):
                        base = 512 * (6 + a_)
                        return ps[:, base + dv1 * u: base + dv1 * (u + 1)]

                    def ACC(a_):
                        return [('ps', 6 + a_)]

                    def acc_all(a_):
                        return ps[:, 512 * (6 + a_): 512 * (6 + a_) + 4 * dv1]
                else:
                    def acc(a_, u):
                        base = 2048 + 1024 * a_ + 512 * (u // 2) + dv1 * (u % 2)
                        return ps[:, base: base + dv1]

                    def ACC(a_):
                        return [('ps', 4 + 2 * a_), ('ps', 5 + 2 * a_)]

                    def acc_all(a_):
                        return ps[:, 2048 + 1024 * a_: 2048 + 1024 * a_ + 512 + 2 * dv1]

                def loads(c):
                    tok = slice(512 * c, 512 * (c + 1))
                    qb = qT[c % 2]
                    for i in range(np_):
                        if nm == 'mla' and i == 1:
                            P.dma('sp', f'k{i}', lambda e: e.dma_start(
                                out=kT[i][:, 0, tok], in_=mx['k'][i][:, tok]), w=[(nm + 'kT', i, c)])
                        else:
                            P.dma('sp', f'k{i}', lambda e: e.dma_start(
                                out=kT[i][:, :, tok], in_=mx['k'][i][:, :, tok].rearrange("h r s -> r h s")),
                                w=[(nm + 'kT', i, c)])
                        P.dma('sp', f'q{i}{c % 2}', lambda e: e.dma_start(
                            out=qb[i][:], in_=mx['q'][i][:, :, tok].rearrange("h r s -> r h s")),
                            w=[(nm + 'qT', c % 2, i)])
                    P.dma('sp', 'vl', lambda e: e.dma_start(
                        out=vv[:, 4 * c:4 * c + 4, :], in_=mx['v'][tok, :].rearrange("(t p) f -> p t f", p=128)),
                        w=[(nm + 'vv', c)])
                    P.dma('sp', f'gl{c % 3}', lambda e: e.dma_start(
                        out=gzt[c % 3][:], in_=gz_s[tok, 512 * G:512 * (G + 1)].rearrange("(t p) f -> p t f", p=128)),
                        w=[('gzt', c % 3)])

                groups = []
                hc = 0
                for c in range(NCH):
                    for h in range(H):
                        full = [(kt, 0, 0) for kt in range(4 * c)]
                        gl = [[(kt, 0, 512 * i) for i, (kt, _, _) in enumerate(full[i0:i0 + GB])]
                              for i0 in range(0, len(full), GB)]
                        if GB == 3:
                            gl.append([(4 * c + 0, 0, 0), (4 * c + 1, 1, 512), (4 * c + 3, 3, 896), (4 * c + 2, 2, 1024)])
                        else:
                            gl.append([(4 * c + 0, 0, 0), (4 * c + 1, 1, 512)])
                            gl.append([(4 * c + 2, 2, 0), (4 * c + 3, 3, 256)])
                        for gi, g in enumerate(gl):
                            groups.append(dict(c=c, h=h, tiles=g, fh=(gi == 0), lh=(gi == len(gl) - 1), aset=hc % 2))
                        hc += 1

                def emit_score(g, slot):
                    c, h = g['c'], g['h']
                    qb = qT[c % 2]
                    if g['fh']:
                        P.op('dve', lambda e: e.memset(acc_all(g['aset']), 0.0), w=ACC(g['aset']))
                    for ti, (kt, u0, off) in enumerate(g['tiles']):
                        b = slot * GB + off // 512
                        lo = 512 * slot * GB + off
                        wd = 512 - 128 * u0
                        diag = kt >= 4 * c
                        for i in range(np_):
                            hk = 0 if (nm == 'mla' and i == 1) else h
                            P.op('pe', lambda e: e.matmul(
                                ps[:, lo:lo + wd], lhsT=kT[i][:, hk, 128 * kt:128 * (kt + 1)],
                                rhs=qb[i][:, h, 128 * u0:512], start=(i == 0),
                                stop=(i == np_ - 1 and not diag), skip_group_check=diag),
                                r=[(nm + 'kT', i, kt // 4), (nm + 'qT', c % 2, i)], w=[PS(b)])
                        if diag:
                            P.op('pe', lambda e: e.matmul(
                                ps[:, lo:lo + 128], lhsT=identb[:], rhs=tri[:],
                                start=False, stop=True, skip_group_check=True),
                                r=['identb', 'tri'], w=[PS(b)])

                def emit_exp(g, slot, pb_):
                    tl = g['tiles']
                    wtot = max(off + 512 - 128 * u0 for (_, u0, off) in tl)
                    lo = 512 * slot * GB
                    banks = sorted(set(slot * GB + off // 512 for (_, _, off) in tl))
                    P.op('act', lambda e: e.activation(
                        out=pT[pb_][:, 0:wtot], in_=ps[:, lo:lo + wtot], func=AF.Exp),
                        r=[PS(b_) for b_ in banks], w=[('pT', pb_)])

                def emit_pv(g, pb_):
                    h, a_ = g['h'], g['aset']
                    for ti, (kt, u0, off) in enumerate(g['tiles']):
                        for u in range(u0, 4):
                            P.op('pe', lambda e: e.matmul(
                                acc(a_, u), lhsT=pT[pb_][:, off + 128 * (u - u0):off + 128 * (u - u0 + 1)],
                                rhs=vv[:, kt, dv1 * h:dv1 * (h + 1)], start=False, stop=False,
                                skip_group_check=True),
                                r=[('pT', pb_), (nm + 'vv', kt // 4)], w=ACC(a_))

                def head_epilogue(g):
                    h, a_ = g['h'], g['aset']
                    for u in range(4):
                        P.op('dve', lambda e: e.reciprocal(
                            out=rcp[:, u:u + 1], in_=acc(a_, u)[:, dv:dv1]), r=ACC(a_), w=['rcp'])
                    for u in range(4):
                        P.op('dve', lambda e: e.tensor_scalar(
                            out=oo[:, u, dv * h:dv * (h + 1)], in0=acc(a_, u)[:, 0:dv],
                            scalar1=rcp[:, u:u + 1], scalar2=None, op0=ALU.mult),
                            r=ACC(a_) + ['rcp'], w=['oo'], ss=True)

                def chunk_epilogue_a(c):
                    for u in range(4):
                        P.op('dve', lambda e: e.tensor_tensor(
                            out=osq[:], in0=oo[:, u, :], in1=oo[:, u, :], op=ALU.mult), r=['oo'], w=['osq'])
                        P.op('dve', lambda e: e.reduce_sum(out=oss[:, u:u + 1], in_=osq[:], axis=AX.X),
                             r=['osq'], w=['oss'])

                def chunk_epilogue_b(c):
                    tok = slice(512 * c, 512 * (c + 1))
                    P.op('act', lambda e: e.activation(
                        out=oss[:], in_=oss[:], func=AF.Ln, bias=epsc[:, 0:1], scale=1.0 / 512),
                        r=['oss', 'epsc'], w=['oss'])
                    P.op('act', lambda e: e.activation(out=ors[:], in_=oss[:], func=AF.Exp, scale=-0.5),
                         r=['oss'], w=['ors'], ss=True)
                    for u in range(4):
                        P.op('dve', lambda e: e.scalar_tensor_tensor(
                            out=yst[:, u, :], in0=oo[:, u, :], scalar=ors[:, u:u + 1], in1=gzt[c % 3][:, u, :],
                            op0=ALU.mult, op1=ALU.mult), r=['oo', 'ors', ('gzt', c % 3)], w=['yst'], ss=True)
                    P.dma('pool', 'yd', lambda e: e.dma_start(
                        out=yy_s[tok, 512 * G:512 * (G + 1)].rearrange("(t p) f -> p t f", p=128), in_=yst[:]),
                        r=['yst'])

                loads(0)
                emit_score(groups[0], 0)
                ng = len(groups)
                pend = []
                for gi, g in enumerate(groups):
                    if pend and pend[0][0] <= gi:
                        chunk_epilogue_b(pend.pop(0)[1])
                    if g['fh'] and g['h'] == 0 and g['c'] + 1 < NCH:
                        loads(g['c'] + 1)
                    if g['fh'] and g['h'] == 0 and nm == 'moba' and 'C' in phases and g['c'] < 6:
                        load_wout(l, range(2 * g['c'], 2 * g['c'] + 2))
                    emit_exp(g, gi % 2, gi % 3)
                    if gi + 1 < ng:
                        emit_score(groups[gi + 1], (gi + 1) % 2)
                    emit_pv(g, gi % 3)
                    if g['lh']:
                        head_epilogue(g)
                        if g['h'] == H - 1:
                            chunk_epilogue_a(g['c'])
                            pend.append((gi + 3, g['c']))
                for _, c_ in pend:
                    chunk_epilogue_b(c_)

        if 'C' in phases:
            P.barrier()
            Cc = Arena(nc, wend, TOPLO)
            wstn = Cc.alloc("wstn", [128, WH], F32)
            yin = Cc.alloc("yin", [128, 4, 1536], BF16)
            yT = Cc.alloc("yT", [128, 12, 512], BF16)
            xin = Cc.alloc("xinC", [128, 4, D], F32)
            x1 = Cc.alloc("x1", [128, 4, D], F32)
            fg = Cc.alloc("fg", [128, D], F32)
            sq = Cc.alloc("sqC", [128, D], F32)
            ssq = Cc.alloc("ssqC", [128, 4], F32)
            rstd = Cc.alloc("rstdC", [128, 4], F32)
            if 'B' not in phases:
                load_wout(l, range(12))
            if not last:
                P.dma('sp', 'wg', lambda e: e.dma_start(out=gcol[:], in_=ln_g[l + 1, :, :]), w=['gcol'])
            WO = [('Wo', k) for k in range(12)]
            if last:
                P.dma('sp', 'wg2', lambda e: e.dma_start(
                    out=fg[:], in_=fin_g.ap().partition_broadcast(128)), w=['fg'])
            pb = [0]
            for c in range(NCH):
                tok = slice(512 * c, 512 * (c + 1))
                if not last:
                    load_win(l + 1, ALLW[2 * c:2 * c + 2], [(wst, 'wst', 'wst'), (wstn, 'wstn', 'wstn')])
                P.dma('sp', 'yl', lambda e, tok=tok: e.dma_start(
                    out=yin[:], in_=yy_s[tok, :].rearrange("(t p) f -> p t f", p=128)),
                    r=[('yy', c, g) for g in range(3)], w=['yin'])
                P.dma('sp', 'xl', lambda e, tok=tok: e.dma_start(
                    out=xin[:], in_=x_src[tok, :].rearrange("(t p) d -> p t d", p=128)), w=['xinC'])
                for k in range(12):
                    b = pb[0] % 4
                    pb[0] += 1
                    for t in range(4):
                        P.op('pe', lambda e, k=k, t=t, b=b: e.transpose(
                            out=psb[:, 1024 * b + 128 * t:1024 * b + 128 * (t + 1)],
                            in_=yin[:, t, 128 * k:128 * (k + 1)], identity=identb[:]),
                            r=['yin', 'identb'], w=[PS(b)])
                    P.op('act' if k % 2 else 'dve',
                         (lambda e, k=k, b=b: e.copy(out=yT[:, k, :], in_=psb[:, 1024 * b:1024 * b + 512])) if k % 2 else
                         (lambda e, k=k, b=b: e.tensor_copy(out=yT[:, k, :], in_=psb[:, 1024 * b:1024 * b + 512])),
                         r=[PS(b)], w=['yT'])
                for t in range(4):
                    for hf in range(2):
                        b = 4 + pb[0] % 4
                        pb[0] += 1
                        for k in range(12):
                            P.op('pe', lambda e, k=k, t=t, hf=hf, b=b: e.matmul(
                                bank(b), lhsT=yT[:, k, 128 * t:128 * (t + 1)], rhs=Wo[:, k, 512 * hf:512 * (hf + 1)],
                                start=(k == 0), stop=(k == 11)), r=WO + ['yT'], w=[PS(b)])
                        P.op('dve', lambda e, t=t, hf=hf, b=b: e.tensor_tensor(
                            out=x1[:, t, 512 * hf:512 * (hf + 1)], in0=bank(b), in1=xin[:, t, 512 * hf:512 * (hf + 1)],
                            op=ALU.add), r=[PS(b), 'xinC'], w=['x1'])
                if not last:
                    P.dma('pool', 'xs', lambda e, tok=tok: e.dma_start(
                        out=xr_s[tok, :].rearrange("(t p) d -> p t d", p=128), in_=x1[:]),
                        r=['x1'], w=[('xr', c)])
                else:
                    for t in range(4):
                        P.op('act', lambda e, t=t: e.activation(out=sq[:], in_=x1[:, t, :], func=AF.Square),
                             r=['x1'], w=['sqC'])
                        P.op('dve', lambda e, t=t: e.reduce_sum(out=ssq[:, t:t + 1], in_=sq[:], axis=AX.X),
                             r=['sqC'], w=['ssqC'])
                    P.op('act', lambda e: e.activation(
                        out=ssq[:], in_=ssq[:], func=AF.Sqrt, bias=epsc[:, 0:1], scale=1.0 / D),
                        r=['ssqC', 'epsc'], w=['ssqC'])
                    P.op('dve', lambda e: e.reciprocal(out=rstd[:], in_=ssq[:]), r=['ssqC'], w=['rstdC'])
                    for t in range(4):
                        P.op('dve', lambda e, t=t: e.scalar_tensor_tensor(
                            out=x1[:, t, :], in0=x1[:, t, :], scalar=rstd[:, t:t + 1], in1=fg[:],
                            op0=ALU.mult, op1=ALU.mult), r=['x1', 'rstdC', 'fg'], w=['x1'], ss=True)
                    P.dma('pool', 'xs', lambda e, tok=tok: e.dma_start(
                        out=y_out[tok, :].rearrange("(t p) d -> p t d", p=128), in_=x1[:]),
                        r=['x1'], w=[('yout', c)])
    P.build()
    return nc


def host_constants():
    bf = ml_dtypes.bfloat16
    f32 = np.float32
    pos = np.arange(S, dtype=f32)
    inv = (f32(10000.0) ** (-np.arange(0, 64, 2, dtype=f32) / f32(64))).astype(f32)
    ang = (pos[:, None] * inv[None, :]).astype(f32)
    cos = np.cos(ang).astype(f32).T
    sin = np.sin(ang).astype(f32).T
    cos64 = np.concatenate([cos, cos], 0)
    sin64 = np.concatenate([-sin, sin], 0)
    c_cos = np.ascontiguousarray(np.concatenate([cos64, cos64], 0))
    c_sin = np.ascontiguousarray(np.concatenate([sin64, sin64], 0))
    kk = np.arange(128)[:, None]
    qq = np.arange(128)[None, :]
    tri = np.where(qq >= kk, 0.0, NEG).astype(bf)

    def split3(v):
        v = v.astype(f32)
        a = v.astype(bf)
        r = (v - a.astype(f32)).astype(f32)
        b = r.astype(bf)
        r2 = (r - b.astype(f32)).astype(f32)
        return np.stack([a, b, r2.astype(bf)], axis=-2)

    slopes = (f32(2.0) ** (-f32(8.0) * np.arange(1, 9, dtype=f32) / f32(8))).astype(f32)
    mp = (slopes[:, None] * pos[None, :]).astype(f32)
    ones = np.ones((8, 3, S), dtype=bf)
    c_mq = np.concatenate([split3(-mp), ones], axis=1)
    onehot = (np.arange(16)[:, None] == (np.arange(S)[None, :] // 256)).astype(f32)
    c_mk = np.concatenate([np.broadcast_to(onehot[None].astype(bf), (8, 16, S)), ones, split3(mp)], axis=1)
    return dict(c_ident=np.eye(128, dtype=f32), c_tri=tri, c_cos=c_cos, c_sin=c_sin,
                c_ones=ones, c_mq=np.ascontiguousarray(c_mq.astype(bf)),
                c_mk=np.ascontiguousarray(c_mk.astype(bf)))


def host_layout(inputs):
    f32 = np.float32
    w_in = np.asarray(inputs["w_in"], dtype=f32)
    kr = w_in[:, :, C_KR:C_KR + 64]
    krs = np.concatenate([kr[:, :, 32:], kr[:, :, :32]], axis=-1)
    w_in_x = np.ascontiguousarray(np.concatenate([w_in, krs], axis=-1))
    w_uq = np.asarray(inputs["mla_w_uq"], dtype=f32).reshape(2, 256, 4, 192)
    nope = w_uq[..., :128].reshape(2, 256, 512)
    rope = w_uq[..., 128:]
    ropes = np.concatenate([rope[..., 32:], rope[..., :32]], axis=-1)
    w_uq_x = np.ascontiguousarray(np.concatenate(
        [nope, rope.reshape(2, 256, 256), ropes.reshape(2, 256, 256)], axis=-1))
    w_ukv = np.asarray(inputs["mla_w_ukv"], dtype=f32).reshape(2, 128, 4, 256)
    w_ukv_x = np.ascontiguousarray(np.concatenate(
        [w_ukv[..., :128].reshape(2, 128, 512), w_ukv[..., 128:].reshape(2, 128, 512)], axis=-1))
    common = dict(
        ln_g=np.ascontiguousarray(np.asarray(inputs["ln_g"], f32).reshape(2, 8, 128).transpose(0, 2, 1)),
        w_in=w_in_x,
        fox_b_f=np.asarray(inputs["fox_b_f"], f32).reshape(2, 8, 1),
        mla_q_g=np.ascontiguousarray(np.asarray(inputs["mla_q_g"], f32).reshape(2, 2, 128).transpose(0, 2, 1)),
        mla_w_uq=w_uq_x, mla_kv_g=np.asarray(inputs["mla_kv_g"], f32).reshape(2, 128, 1),
        mla_w_ukv=w_ukv_x,
        out_g=np.ascontiguousarray(np.asarray(inputs["out_g"], f32).reshape(2, 12, 128).transpose(0, 2, 1)),
        w_out=np.asarray(inputs["w_out"], f32),
        final_g=np.asarray(inputs["final_g"], f32))
    common.update(host_constants())
    return common


_NC_CACHE = {}


def kernel(**inputs):
    x = np.asarray(inputs["x"], dtype=np.float32)
    common = host_layout(inputs)
    if 'nc' not in _NC_CACHE:
        _NC_CACHE['nc'] = build_program()
    nc = _NC_CACHE['nc']
    in_maps = [dict(common, x=np.ascontiguousarray(x[b])) for b in range(8)]
    res = run_bass_kernel_spmd(nc, in_maps, core_ids=list(range(8)))
    return np.stack([np.asarray(r["y"], dtype=np.float32) for r in res.results], axis=0)
```
